# Optimizing a Trainium2 kernel written in Bass

```python
import math
import numpy as np
import jax
import jax.numpy as jnp
from jax import lax

D_MODEL = 4096
BATCH = 1
SEQ = 8192
DEPTH = 2

GRID_W = 64
CTX_LEN = 256
N_EVEN = (DEPTH + 1) // 2
N_ODD = DEPTH // 2
MIX_W = D_MODEL
ADA_CHUNKS = 6
NORM_EPS = 1e-6

SSD_W = MIX_W // 2
SSD_P = 64
SSD_HEADS = SSD_W // SSD_P
SSD_G = 4
SSD_N = 128
SSD_CONV = 4
SSD_CHUNK = 128
XBC_W = SSD_W + 2 * SSD_G * SSD_N

NA_W = MIX_W - SSD_W
NA_DH = 128
NA_HEADS = NA_W // NA_DH
NA_WIN_H = 8
NA_WIN_W = 16
NA_QCOLS = 16
NA_KCOLS = NA_QCOLS + NA_WIN_W

EV_IN_COLS = SSD_W + XBC_W + 2 * SSD_HEADS + 3 * NA_W
FF_DENSE = 11008

RG_W = MIX_W // 2
RG_BLOCKS = 16
RG_BW = RG_W // RG_BLOCKS
RG_CONV = 4
RG_C = 8.0

HY_W = MIX_W - RG_W
HY_ORDER = 2
HY_CONV = 3
HY_EMB = 33
HY_BANDS = (HY_EMB - 1) // 2
HY_FFN = 64
HY_INNER = 2
HY_FILT = HY_ORDER * 2 * HY_W
HY_TARGET = 1e-2
HY_PCT_SHORT = 0.3
HY_PCT_LONG = 1.5
OD_IN_COLS = 2 * RG_W + 3 * HY_W

N_EXPERTS = 8
TOP_K = 2
FF_EXPERT = 3584

kernel_name = 'hybrid_ssd_natten_rglru_hyena_moe_dit'


def _rmsnorm(x, w):
    xf = x.astype(jnp.float32)
    y = xf * lax.rsqrt(jnp.mean(xf * xf, axis=-1, keepdims=True) + NORM_EPS)
    return (y * w.astype(jnp.float32)).astype(x.dtype)


def _modulated_norm(x, w, shift, scale):
    return _rmsnorm(x, w) * (1 + scale[:, None]) + shift[:, None]


def _dwconv(u, w, b, pad_left):
    k, ch = w.shape
    y = lax.conv_general_dilated(u, w.astype(u.dtype)[:, None, :], window_strides=(1,),
                                 padding=[(pad_left, k - 1 - pad_left)],
                                 dimension_numbers=('NWC', 'WIO', 'NWC'), feature_group_count=ch)
    return y + b.astype(u.dtype)


def _swiglu(h, w1, w3, w2):
    return (jax.nn.silu(h @ w1) * (h @ w3)) @ w2


def _segsum(a):
    t = a.shape[-1]
    ab = jnp.broadcast_to(a[..., :, None], a.shape + (t,))
    strict = jnp.tril(jnp.ones((t, t), dtype=bool), -1)
    cs = jnp.cumsum(jnp.where(strict, ab, 0.0), axis=-2)
    return jnp.where(jnp.tril(jnp.ones((t, t), dtype=bool)), cs, -jnp.inf)


def _ssd_chunked(xs, dt, a, bm, cm, h0):
    b, L, H, P = xs.shape
    G, N = bm.shape[2], bm.shape[3]
    E = H // G
    Q = SSD_CHUNK
    nc = L // Q
    xd = (xs.astype(jnp.float32) * dt[..., None]).reshape(b, nc, Q, G, E, P)
    da = (dt * a).reshape(b, nc, Q, G, E).transpose(0, 3, 4, 1, 2)
    bc = bm.astype(jnp.float32).reshape(b, nc, Q, G, N)
    cc = cm.astype(jnp.float32).reshape(b, nc, Q, G, N)
    da_cum = jnp.cumsum(da, axis=-1)
    decay_in = jnp.exp(_segsum(da))
    cb = jnp.einsum('bclgn,bcsgn->bgcls', cc, bc)
    y_diag = jnp.einsum('bgcls,bgecls,bcsgep->bclgep', cb, decay_in, xd)
    decay_to_end = jnp.exp(da_cum[..., -1:] - da_cum)
    chunk_states = jnp.einsum('bclgn,bgecl,bclgep->bcgepn', bc, decay_to_end, xd)
    states = jnp.concatenate([h0.reshape(b, 1, G, E, P, N), chunk_states], axis=1)
    chunk_decay = jnp.exp(_segsum(jnp.pad(da_cum[..., -1], [(0, 0), (0, 0), (0, 0), (1, 0)])))
    states = jnp.einsum('bgezc,bcgepn->bzgepn', chunk_decay, states)
    y_off = jnp.einsum('bclgn,bcgepn,bgecl->bclgep', cc, states[:, :-1], jnp.exp(da_cum))
    y = (y_diag + y_off).reshape(b, L, H, P)
    return y, states[:, -1].reshape(b, H, P, N)


def _ssd_mixer(z, xbc, dt_raw, conv_w, conv_b, dt_bias, a_log, d_skip, norm_w, h0_fwd, h0_bwd):
    b, L, _ = z.shape
    xbc = jax.nn.silu(_dwconv(xbc, conv_w, conv_b, SSD_CONV // 2))
    gn = SSD_G * SSD_N
    xs = xbc[..., :SSD_W].reshape(b, L, SSD_HEADS, SSD_P)
    bm = xbc[..., SSD_W:SSD_W + gn].reshape(b, L, SSD_G, SSD_N)
    cm = xbc[..., SSD_W + gn:].reshape(b, L, SSD_G, SSD_N)
    dt = jax.nn.softplus(dt_raw.astype(jnp.float32).reshape(b, L, 2, SSD_HEADS)
                         + dt_bias.astype(jnp.float32))
    a = -jnp.exp(a_log.astype(jnp.float32))
    y_f, s_f = _ssd_chunked(xs, dt[:, :, 0], a[0], bm, cm, h0_fwd)
    y_b, s_b = _ssd_chunked(xs[:, ::-1], dt[:, ::-1, 1], a[1], bm[:, ::-1], cm[:, ::-1], h0_bwd)
    y = y_f + y_b[:, ::-1] + xs.astype(jnp.float32) * d_skip.astype(jnp.float32)[:, None]
    y = y.reshape(b, L, SSD_W) * jax.nn.silu(z.astype(jnp.float32))
    yg = y.reshape(b, L, SSD_G, SSD_W // SSD_G)
    yg = yg * lax.rsqrt(jnp.mean(yg * yg, axis=-1, keepdims=True) + NORM_EPS)
    y = yg.reshape(b, L, SSD_W) * norm_w.astype(jnp.float32)
    return y.astype(z.dtype), s_f, s_b


def _context_attention(q, k, v):
    s = jnp.einsum('bqhd,bkhd->bhqk', q, k, preferred_element_type=jnp.float32) * NA_DH ** -0.5
    p = jax.nn.softmax(s, axis=-1).astype(v.dtype)
    o = jnp.einsum('bhqk,bkhd->bqhd', p, v)
    return o.reshape(q.shape[0], q.shape[1], NA_W)


def _neighbourhood_attention(q, k, v, k_ctx, v_ctx, rpb):
    b, L, H, dh = q.shape
    rows = L // GRID_W
    kh = min(NA_WIN_H, rows)
    n_cb = GRID_W // NA_QCOLS
    scale = dh ** -0.5
    qg = q.reshape(b, rows, GRID_W, H, dh)
    kg = k.reshape(b, rows, GRID_W, H, dh)
    vg = v.reshape(b, rows, GRID_W, H, dh)
    jb = np.arange(n_cb)[:, None]
    qcols = jb * NA_QCOLS + np.arange(NA_QCOLS)[None]
    kcols = (np.clip(jb * NA_QCOLS - NA_WIN_W // 2, 0, GRID_W - NA_KCOLS)
             + np.arange(NA_KCOLS)[None])
    cstart = np.clip(qcols - NA_WIN_W // 2, 0, GRID_W - NA_WIN_W)
    col_ok = ((kcols[:, None, :] >= cstart[..., None])
              & (kcols[:, None, :] < cstart[..., None] + NA_WIN_W))
    dcol = np.clip(kcols[:, None, :] - qcols[..., None] + NA_WIN_W - 1, 0, 2 * NA_WIN_W - 2)
    col_bias = rpb.astype(jnp.float32)[:, :, dcol]
    n_loc = kh * NA_KCOLS

    def row_block(r):
        rs = jnp.clip(r - kh // 2, 0, rows - kh)
        q_r = lax.dynamic_index_in_dim(qg, r, axis=1, keepdims=False).reshape(b, n_cb, NA_QCOLS, H, dh)
        k_blk = lax.dynamic_slice_in_dim(kg, rs, kh, axis=1)[:, :, kcols]
        v_blk = lax.dynamic_slice_in_dim(vg, rs, kh, axis=1)[:, :, kcols]
        drow = rs + jnp.arange(kh) - r + NA_WIN_H - 1
        bias = jnp.take(col_bias, drow, axis=1).transpose(0, 2, 3, 1, 4)
        s = jnp.einsum('bjqhd,bajkhd->bhjqak', q_r, k_blk,
                       preferred_element_type=jnp.float32) * scale + bias
        s = jnp.where(col_ok[None, None, :, :, None, :], s, -jnp.inf)
        s = s.reshape(b, H, n_cb, NA_QCOLS, n_loc)
        s_ctx = jnp.einsum('bjqhd,bchd->bhjqc', q_r, k_ctx,
                           preferred_element_type=jnp.float32) * scale
        p = jax.nn.softmax(jnp.concatenate([s, s_ctx], axis=-1), axis=-1).astype(v.dtype)
        p_loc = p[..., :n_loc].reshape(b, H, n_cb, NA_QCOLS, kh, NA_KCOLS)
        o = (jnp.einsum('bhjqak,bajkhd->bjqhd', p_loc, v_blk)
             + jnp.einsum('bhjqc,bchd->bjqhd', p[..., n_loc:], v_ctx))
        return o.reshape(b, GRID_W, H, dh)

    out = lax.map(row_block, jnp.arange(rows))
    return jnp.moveaxis(out, 0, 1).reshape(b, L, H * dh)


def _even_mixer(h, hc, in_w, conv_w, conv_b, dt_bias, a_log, d_skip, norm_w, rpb, out_w, need_ctx):
    o1 = SSD_W
    o2 = o1 + XBC_W
    o3 = o2 + 2 * SSD_HEADS
    splits = [o1, o2, o3, o3 + NA_W, o3 + 2 * NA_W]

    def heads(t):
        return t.reshape(t.shape[0], t.shape[1], NA_HEADS, NA_DH)

    ssd_params = (conv_w, conv_b, dt_bias, a_log, d_skip, norm_w)
    zc, xbcc, dtc, qc, kc, vc = jnp.split(hc @ in_w, splits, axis=-1)
    zero = jnp.zeros((hc.shape[0], SSD_HEADS, SSD_P, SSD_N), jnp.float32)
    y_ssd_c, s_fwd, s_bwd = _ssd_mixer(zc, xbcc, dtc, *ssd_params, zero, zero)
    kc, vc = heads(kc), heads(vc)
    z, xbc, dt_raw, q, k, v = jnp.split(h @ in_w, splits, axis=-1)
    y_ssd, _, _ = _ssd_mixer(z, xbc, dt_raw, *ssd_params, s_fwd, s_bwd)
    y_na = _neighbourhood_attention(heads(q), heads(k), heads(v), kc, vc, rpb)
    mix = jnp.concatenate([y_ssd, y_na], axis=-1) @ out_w
    mix_c = None
    if need_ctx:
        y_na_c = _context_attention(heads(qc), kc, vc)
        mix_c = jnp.concatenate([y_ssd_c, y_na_c], axis=-1) @ out_w
    return mix, mix_c


def _linear_scan(a, b, h0):
    b = b.at[:, 0].add(a[:, 0] * h0)
    _, h = lax.associative_scan(lambda l, r: (l[0] * r[0], r[0] * l[1] + r[1]), (a, b), axis=1)
    return h


def _rglru_scans(u, gate_w, gate_b, lam, h0_f, h0_b):
    b, L, _ = u.shape
    uf = u.astype(jnp.float32)
    g = jnp.einsum('blnc,dgncf->bldgnf', uf.reshape(b, L, RG_BLOCKS, RG_BW),
                   gate_w.astype(jnp.float32)) + gate_b.astype(jnp.float32)
    g = jax.nn.sigmoid(g).reshape(b, L, 2, 2, RG_W)
    r, i = g[:, :, :, 0], g[:, :, :, 1]
    log_a = RG_C * r * jax.nn.log_sigmoid(lam.astype(jnp.float32))
    a = jnp.exp(log_a)
    inp = jnp.sqrt(-jnp.expm1(2.0 * log_a)) * i * uf[:, :, None]
    h_f = _linear_scan(a[:, :, 0], inp[:, :, 0], h0_f)
    h_b = _linear_scan(a[:, ::-1, 1], inp[:, ::-1, 1], h0_b)[:, ::-1]
    return h_f, h_b


def _hyena_filters(L, w_in, b_in, w_mid, b_mid, w_out, freq, deltas):
    f32 = jnp.float32
    t = jnp.linspace(0.0, 1.0, L, dtype=f32)[:, None]
    ang = ((2.0 * math.pi / L) * jnp.arange(L, dtype=f32)[:, None]
           * jnp.linspace(1e-4, HY_BANDS - 1, HY_BANDS, dtype=f32)[None])
    feats = jnp.concatenate([t, jnp.cos(ang), -jnp.sin(ang)], axis=-1)
    freq = freq.astype(f32)
    hid = jnp.sin(freq * (feats @ w_in.astype(f32) + b_in.astype(f32)))
    for m in range(HY_INNER):
        hid = jnp.sin(freq * (hid @ w_mid[m].astype(f32) + b_mid[m].astype(f32)))
    filt = (hid @ w_out.astype(f32)) * jnp.exp(-t * jnp.abs(deltas.astype(f32)))
    return filt.reshape(L, HY_ORDER, 2, HY_W)


def _bidir_long_conv(u, f_fwd, f_bwd, skip):
    L, w = f_fwd.shape
    circ = jnp.concatenate([f_fwd[:1] + f_bwd[:1], f_fwd[1:], jnp.zeros((1, w), f_fwd.dtype),
                            f_bwd[:0:-1]], axis=0)
    y = jnp.fft.irfft(jnp.fft.rfft(u, n=2 * L, axis=1) * jnp.fft.rfft(circ, axis=0),
                      n=2 * L, axis=1)[:, :L]
    return y + u * skip


def _hyena(p, conv_w, conv_b, w_in, b_in, w_mid, b_mid, w_out, freq, deltas, skip):
    L = p.shape[1]
    u = _dwconv(p, conv_w, conv_b, (HY_CONV - 1) // 2).astype(jnp.float32)
    x1, x2, z = jnp.split(u, 3, axis=-1)
    filt = _hyena_filters(L, w_in, b_in, w_mid, b_mid, w_out, freq, deltas)
    for n, gate in enumerate((x1, x2)):
        z = gate * _bidir_long_conv(z, filt[:, n, 0], filt[:, n, 1], skip[n].astype(jnp.float32))
    return z.astype(p.dtype)


def _odd_mixer(h, hc, in_w, conv_w, conv_b, gate_w, gate_b, lam, hy_params, out_w, need_ctx):
    splits = [RG_W, 2 * RG_W]
    rg = (gate_w, gate_b, lam)
    pad = RG_CONV // 2
    zero = jnp.zeros((hc.shape[0], RG_W), jnp.float32)
    if need_ctx:
        gate_c, rec_c, hy_c = jnp.split(hc @ in_w, splits, axis=-1)
    else:
        rec_c = hc @ in_w[:, RG_W:2 * RG_W]
    hf_c, hb_c = _rglru_scans(_dwconv(rec_c, conv_w, conv_b, pad), *rg, zero, zero)
    gate, rec, hy = jnp.split(h @ in_w, splits, axis=-1)
    hf, hb = _rglru_scans(_dwconv(rec, conv_w, conv_b, pad), *rg, hf_c[:, -1], hb_c[:, 0])
    y_rg = jax.nn.gelu(gate) * (hf + hb).astype(gate.dtype)
    mix = jnp.concatenate([y_rg, _hyena(hy, *hy_params)], axis=-1) @ out_w
    mix_c = None
    if need_ctx:
        y_rg_c = jax.nn.gelu(gate_c) * (hf_c + hb_c).astype(gate_c.dtype)
        mix_c = jnp.concatenate([y_rg_c, _hyena(hy_c, *hy_params)], axis=-1) @ out_w
    return mix, mix_c


def _moe(h, router_w, w1, w3, w2):
    logits = jnp.einsum('bld,de->ble', h, router_w, preferred_element_type=jnp.float32)
    top_v, top_i = lax.top_k(logits, TOP_K)
    top_p = jax.nn.softmax(top_v, axis=-1)
    gates = jnp.sum(jax.nn.one_hot(top_i, N_EXPERTS, dtype=jnp.float32) * top_p[..., None], axis=-2)
    y = jnp.zeros_like(h)
    for e in range(N_EXPERTS):
        y = y + gates[..., e:e + 1].astype(h.dtype) * _swiglu(h, w1[e], w3[e], w2[e])
    return y


def setup_inputs(seed: int = 0) -> dict:
    key = jax.random.key(seed)
    ks = iter(jax.random.split(key, 64))
    f32 = jnp.float32

    def nrm(shape, std):
        return std * jax.random.normal(next(ks), shape, f32)

    def unif(shape, lo, hi):
        return jax.random.uniform(next(ks), shape, f32, lo, hi)

    dt0 = jnp.exp(unif((N_EVEN, 2, SSD_HEADS), math.log(1e-3), math.log(1e-1)))
    a_base = unif((N_ODD, 2, RG_W), 0.9, 0.999) ** (1.0 / RG_C)
    decay_lo = -math.log(HY_TARGET) / HY_PCT_LONG
    decay_hi = -math.log(HY_TARGET) / HY_PCT_SHORT
    base = jnp.broadcast_to(jnp.linspace(decay_lo, decay_hi, HY_W, dtype=f32), (HY_ORDER * 2, HY_W))
    deltas = base.reshape(1, HY_FILT) * (1.0 + nrm((N_ODD, HY_FILT), 0.05))
    return {
        'x': nrm((BATCH, SEQ, D_MODEL), 1.0),
        'c': nrm((BATCH, D_MODEL), 1.0),
        'ctx': nrm((BATCH, CTX_LEN, D_MODEL), 1.0),
        'c_ctx': nrm((D_MODEL,), 1.0),
        'ada_w': nrm((DEPTH, D_MODEL, ADA_CHUNKS * D_MODEL), 0.5 * D_MODEL ** -0.5),
        'ada_b': nrm((DEPTH, ADA_CHUNKS * D_MODEL), 0.01),
        'norm_mix_w': 1.0 + nrm((DEPTH, D_MODEL), 0.02),
        'norm_ffn_w': 1.0 + nrm((DEPTH, D_MODEL), 0.02),
        'norm_out_w': 1.0 + nrm((D_MODEL,), 0.02),
        'ev_in_w': nrm((N_EVEN, D_MODEL, EV_IN_COLS), D_MODEL ** -0.5),
        'ev_ssd_conv_w': nrm((N_EVEN, SSD_CONV, XBC_W), SSD_CONV ** -0.5),
        'ev_ssd_conv_b': nrm((N_EVEN, XBC_W), 0.01),
        'ev_ssd_dt_bias': dt0 + jnp.log(-jnp.expm1(-dt0)),
        'ev_ssd_a_log': jnp.log(unif((N_EVEN, 2, SSD_HEADS), 1.0, 16.0)),
        'ev_ssd_d': 1.0 + nrm((N_EVEN, SSD_HEADS), 0.1),
        'ev_ssd_norm_w': 1.0 + nrm((N_EVEN, SSD_W), 0.02),
        'ev_na_rpb': nrm((N_EVEN, NA_HEADS, 2 * NA_WIN_H - 1, 2 * NA_WIN_W - 1), 0.1),
        'ev_out_w': nrm((N_EVEN, MIX_W, D_MODEL), MIX_W ** -0.5),
        'ev_ffn_w1': nrm((N_EVEN, D_MODEL, FF_DENSE), D_MODEL ** -0.5),
        'ev_ffn_w3': nrm((N_EVEN, D_MODEL, FF_DENSE), D_MODEL ** -0.5),
        'ev_ffn_w2': nrm((N_EVEN, FF_DENSE, D_MODEL), FF_DENSE ** -0.5),
        'od_in_w': nrm((N_ODD, D_MODEL, OD_IN_COLS), D_MODEL ** -0.5),
        'od_rg_conv_w': nrm((N_ODD, RG_CONV, RG_W), RG_CONV ** -0.5),
        'od_rg_conv_b': nrm((N_ODD, RG_W), 0.01),
        'od_rg_gate_w': nrm((N_ODD, 2, 2, RG_BLOCKS, RG_BW, RG_BW), RG_BW ** -0.5),
        'od_rg_gate_b': nrm((N_ODD, 2, 2, RG_BLOCKS, RG_BW), 0.01),
        'od_rg_lambda': jnp.log(a_base) - jnp.log1p(-a_base),
        'od_hy_conv_w': nrm((N_ODD, HY_CONV, 3 * HY_W), HY_CONV ** -0.5),
        'od_hy_conv_b': nrm((N_ODD, 3 * HY_W), 0.01),
        'od_hy_w_in': nrm((N_ODD, HY_EMB, HY_FFN), HY_EMB ** -0.5),
        'od_hy_b_in': nrm((N_ODD, HY_FFN), 0.1),
        'od_hy_w_mid': nrm((N_ODD, HY_INNER, HY_FFN, HY_FFN), HY_FFN ** -0.5),
        'od_hy_b_mid': nrm((N_ODD, HY_INNER, HY_FFN), 0.1),
        'od_hy_w_out': nrm((N_ODD, HY_FFN, HY_FILT), 0.05 * HY_FFN ** -0.5),
        'od_hy_freq': 1.0 + nrm((N_ODD, HY_FFN), 0.1),
        'od_hy_deltas': deltas,
        'od_hy_skip': nrm((N_ODD, HY_ORDER, HY_W), 0.5),
        'od_out_w': nrm((N_ODD, MIX_W, D_MODEL), MIX_W ** -0.5),
        'od_router_w': nrm((N_ODD, D_MODEL, N_EXPERTS), D_MODEL ** -0.5),
        'od_moe_w1': nrm((N_ODD, N_EXPERTS, D_MODEL, FF_EXPERT), D_MODEL ** -0.5),
        'od_moe_w3': nrm((N_ODD, N_EXPERTS, D_MODEL, FF_EXPERT), D_MODEL ** -0.5),
        'od_moe_w2': nrm((N_ODD, N_EXPERTS, FF_EXPERT, D_MODEL), FF_EXPERT ** -0.5),
    }


def reference(x, c, ctx, c_ctx, ada_w, ada_b, norm_mix_w, norm_ffn_w, norm_out_w,
              ev_in_w, ev_ssd_conv_w, ev_ssd_conv_b, ev_ssd_dt_bias, ev_ssd_a_log, ev_ssd_d,
              ev_ssd_norm_w, ev_na_rpb, ev_out_w, ev_ffn_w1, ev_ffn_w3, ev_ffn_w2,
              od_in_w, od_rg_conv_w, od_rg_conv_b, od_rg_gate_w, od_rg_gate_b, od_rg_lambda,
              od_hy_conv_w, od_hy_conv_b, od_hy_w_in, od_hy_b_in, od_hy_w_mid, od_hy_b_mid,
              od_hy_w_out, od_hy_freq, od_hy_deltas, od_hy_skip, od_out_w,
              od_router_w, od_moe_w1, od_moe_w3, od_moe_w2):
    for i in range(DEPTH):
        j = i // 2
        need_ctx = i < DEPTH - 1
        mod = jnp.split(jax.nn.silu(c) @ ada_w[i] + ada_b[i], ADA_CHUNKS, axis=-1)
        mod_c = jnp.split(jax.nn.silu(c_ctx)[None] @ ada_w[i] + ada_b[i], ADA_CHUNKS, axis=-1)
        h = _modulated_norm(x, norm_mix_w[i], mod[0], mod[1])
        hc = _modulated_norm(ctx, norm_mix_w[i], mod_c[0], mod_c[1])
        if i % 2 == 0:
            mix, mix_c = _even_mixer(h, hc, ev_in_w[j], ev_ssd_conv_w[j], ev_ssd_conv_b[j],
                                     ev_ssd_dt_bias[j], ev_ssd_a_log[j], ev_ssd_d[j],
                                     ev_ssd_norm_w[j], ev_na_rpb[j], ev_out_w[j], need_ctx)
            ffn = lambda t: _swiglu(t, ev_ffn_w1[j], ev_ffn_w3[j], ev_ffn_w2[j])
        else:
            hy_params = (od_hy_conv_w[j], od_hy_conv_b[j], od_hy_w_in[j], od_hy_b_in[j],
                         od_hy_w_mid[j], od_hy_b_mid[j], od_hy_w_out[j], od_hy_freq[j],
                         od_hy_deltas[j], od_hy_skip[j])
            mix, mix_c = _odd_mixer(h, hc, od_in_w[j], od_rg_conv_w[j], od_rg_conv_b[j],
                                    od_rg_gate_w[j], od_rg_gate_b[j], od_rg_lambda[j],
                                    hy_params, od_out_w[j], need_ctx)
            ffn = lambda t: _moe(t, od_router_w[j], od_moe_w1[j], od_moe_w3[j], od_moe_w2[j])
        x = x + mod[2][:, None] * mix
        x = x + mod[5][:, None] * ffn(_modulated_norm(x, norm_ffn_w[i], mod[3], mod[4]))
        if need_ctx:
            ctx = ctx + mod_c[2][:, None] * mix_c
            ctx = ctx + mod_c[5][:, None] * ffn(_modulated_norm(ctx, norm_ffn_w[i], mod_c[3], mod_c[4]))
    return _rmsnorm(x, norm_out_w)
```

```python
import math
import contextlib
import types
import numpy as np
import concourse.bass as bass
import concourse.mybir as mybir
from concourse.bass_utils import run_bass_kernel_spmd

AF = mybir.ActivationFunctionType
ALU = mybir.AluOpType
F32 = mybir.dt.float32
BF16 = mybir.dt.bfloat16
AX = mybir.AxisListType

SEM_EPOCH = 30000


class Buf:
    def __init__(self, prog, t, name):
        self.p = prog
        self.t = t
        self.name = name
        self.last_w = None
        self.reads = []
        self.dsem = None
        self.dcount = 0

    def __getitem__(self, idx):
        return self.t[idx]


class Prog:
    def __init__(self):
        self.nc = bass.Bass("TRN2", target_bir_lowering=False)
        self.es = contextlib.ExitStack()
        nc = self.nc
        self.eng = {"pe": nc.tensor, "dve": nc.vector, "act": nc.scalar,
                    "pool": nc.gpsimd, "sp": nc.sync}
        self.cnt = {k: 0 for k in self.eng}
        self.esems = {k: [] for k in self.eng}
        self.sems = {}
        self.waited = {k: {} for k in self.eng}
        self.nbuf = 0
        self.dma_out_events = []
        self.n_inst = 0
        self.ops = []

    def _sem(self, key):
        if key not in self.sems:
            self.sems[key] = self.es.enter_context(self.nc.semaphore(key))
        return self.sems[key]

    def sb(self, shape, dt=F32, name=None):
        self.nbuf += 1
        name = "s_" + (name or f"sb{self.nbuf}")
        t = self.es.enter_context(self.nc.sbuf_tensor(name, list(shape), dt))
        return Buf(self, t, name)

    def ps(self, shape, dt=F32, name=None):
        self.nbuf += 1
        name = "p_" + (name or f"ps{self.nbuf}")
        t = self.es.enter_context(self.nc.psum_tensor(name, list(shape), dt))
        b = Buf(self, t, name)
        b.excl = True
        return b

    def din(self, name, shape, dt=F32):
        return self.nc.dram_tensor(name, list(shape), dt, kind="ExternalInput").ap()

    def dout(self, name, shape, dt=F32):
        return self.nc.dram_tensor(name, list(shape), dt, kind="ExternalOutput").ap()

    def op(self, eng, fn, w=(), r=()):
        self.ops.append(dict(kind="op", eng=eng, fn=_freeze(fn), w=list(w), r=list(r)))

    def dma(self, q, out, in_, w=(), r=(), is_output=False, **kw):
        if not (w or r):
            raise ValueError("dma needs a tracked buffer")
        self.ops.append(dict(kind="dma", eng=q, out=out, in_=in_, w=list(w), r=list(r), is_output=is_output, kw=kw))

    def finish(self):
        ops = self.ops
        last_w = {}
        reads = {}
        deps = []
        for i, o in enumerate(ops):
            d = set()
            for b in o["r"]:
                if id(b) in last_w:
                    d.add(last_w[id(b)])
                if getattr(b, "excl", False):
                    d.update(reads.get(id(b), ()))
            for b in o["w"]:
                if id(b) in last_w:
                    d.add(last_w[id(b)])
                d.update(reads.get(id(b), ()))
            d.discard(i)
            deps.append(d)
            for b in o["w"]:
                last_w[id(b)] = i
                reads[id(b)] = []
            for b in o["r"]:
                if b not in o["w"]:
                    reads.setdefault(id(b), []).append(i)
        needs_inc = [False] * len(ops)
        for i, d in enumerate(deps):
            for j in d:
                if ops[i]["eng"] == "pe" and ops[j]["kind"] == "op" and ops[j]["eng"] == "pe":
                    continue
                needs_inc[j] = True
        events = [None] * len(ops)
        cnt = {k: 0 for k in self.eng}
        waited = {k: {} for k in self.eng}
        out_events = []
        for i, o in enumerate(ops):
            eng = o["eng"]
            E = self.eng[eng]
            need = {}
            for j in deps[i]:
                ev = events[j]
                if ev is None:
                    continue
                k, v = ev
                if need.get(k, 0) < v:
                    need[k] = v
            wd = waited[eng]
            for k, v in need.items():
                if wd.get(k, 0) >= v:
                    continue
                E.wait_ge(self._sem(k), v)
                wd[k] = v
                self.n_inst += 1
            if o["kind"] == "op":
                ins = o["fn"](E)
                if needs_inc[i]:
                    c = cnt[eng]
                    key = f"e_{eng}_{c // SEM_EPOCH}"
                    ins.then_inc(self._sem(key), 1)
                    cnt[eng] = c + 1
                    events[i] = (key, c % SEM_EPOCH + 1)
            else:
                owner = (o["w"] + o["r"])[0]
                if owner.dsem is None:
                    owner.dsem = f"d_{owner.name}"
                ins = E.dma_start(out=o["out"], in_=o["in_"], **o["kw"])
                owner.dcount += 16
                ins.then_inc(self._sem(owner.dsem), 16)
                events[i] = (owner.dsem, owner.dcount)
                if o["is_output"]:
                    out_events.append(events[i])
            self.n_inst += 1
        need = {}
        for k, v in out_events:
            if need.get(k, 0) < v:
                need[k] = v
        for k, v in need.items():
            if waited["sp"].get(k, 0) < v:
                self.eng["sp"].wait_ge(self._sem(k), v)
        self.n_incs = dict(cnt)
        self.es.close()
        return self.nc


def _freeze(fn):
    if fn.__closure__ is None:
        return fn
    cells = []
    for c in fn.__closure__:
        try:
            cells.append(types.CellType(c.cell_contents))
        except ValueError:
            cells.append(c)
    return types.FunctionType(fn.__code__, fn.__globals__, fn.__name__, fn.__defaults__, tuple(cells))


def run(prog_nc, in_maps, n=8):
    res = run_bass_kernel_spmd(prog_nc, in_maps, core_ids=list(range(n)))
    return res.results


D = 4096
KC = D // 128
EPS = 1e-6


def fm(v):
    return np.ascontiguousarray(np.asarray(v, np.float32).reshape(KC, 128).T)


def build_ada(ncols=3072, nl=2):
    p = Prog()
    cc = p.din("cc", [128, KC, 2])
    aw = p.din("aw", [nl, D, ncols])
    ab = p.din("ab", [nl, 2, ncols])
    out = p.dout("mod", [nl, 2, ncols])
    c32 = p.sb([128, KC, 2], F32, "c32")
    cb = p.sb([128, KC, 2], BF16, "cb")
    p.dma("sp", c32[:, :, :], cc, w=[c32])
    p.op("act", lambda e: e.activation(out=cb[:, :, :], in_=c32[:, :, :], func=AF.Silu), w=[cb], r=[c32])
    CT = 512
    wts = [p.sb([128, KC, CT], BF16, f"wt{i}") for i in range(2)]
    accs = [p.ps([2, CT], F32, f"acc{i}") for i in range(2)]
    bts = [p.sb([2, CT], F32, f"bt{i}") for i in range(2)]
    ots = [p.sb([2, CT], F32, f"ot{i}") for i in range(2)]
    it = 0
    for l in range(nl):
        for ct in range(ncols // CT):
            wt, acc, bt, ot = wts[it % 2], accs[it % 2], bts[it % 2], ots[it % 2]
            it += 1
            cs = slice(ct * CT, (ct + 1) * CT)
            p.dma("pool", wt[:, :, :], aw[l, :, cs].rearrange("(k p) n -> p k n", p=128), w=[wt])
            p.dma("sp", bt[:, :], ab[l, :, cs], w=[bt])
            for k in range(KC):
                p.op("pe", lambda e: e.matmul(acc[:, :], lhsT=cb[:, k, :], rhs=wt[:, k, :],
                                              start=(k == 0), stop=(k == KC - 1)), w=[acc], r=[cb, wt])
            p.op("dve", lambda e: e.tensor_tensor(out=ot[:, :], in0=acc[:, :], in1=bt[:, :], op=ALU.add),
                 w=[ot], r=[acc, bt])
            p.dma("sp", out[l, :, cs], ot[:, :], r=[ot], is_output=True)
    return p.finish()


def run_ada(c, c_ctx, ada_w, ada_b):
    nl = ada_w.shape[0]
    ncols = ada_w.shape[2] // 8
    cc = np.stack([fm(c.reshape(-1)), fm(c_ctx.reshape(-1))], axis=-1)
    nc = build_ada(ncols, nl)
    maps = []
    for i in range(8):
        cs = slice(i * ncols, (i + 1) * ncols)
        maps.append({"cc": cc, "aw": np.ascontiguousarray(ada_w[:, :, cs]),
                     "ab": np.ascontiguousarray(np.broadcast_to(ada_b[:, None, cs], (nl, 2, ncols)))})
    res = run(nc, maps)
    return np.concatenate([r["mod"] for r in res], axis=-1)


class NormCtx:
    def __init__(self, p):
        self.p = p
        self.ones = p.sb([128, 128], F32, "ones")
        p.op("dve", lambda e: e.memset(self.ones[:, :], 1.0), w=[self.ones])
        self.sq = [p.sb([128, 512], F32, f"nsq{i}") for i in range(2)]
        self.ss = p.ps([128, 512], F32, "nss")
        self.rstd = p.sb([128, 512], F32, "nrstd")
        self.tmp = [p.sb([128, 512], F32, f"ntmp{i}") for i in range(2)]

    def stats(self, xs, mt):
        p = self.p
        for k in range(KC):
            sq = self.sq[k % 2]
            p.op("act", lambda e: e.activation(out=sq[:, :mt], in_=xs[:, k, :mt], func=AF.Square), w=[sq], r=[xs])
            p.op("pe", lambda e: e.matmul(self.ss[:, :mt], lhsT=self.ones[:, :], rhs=sq[:, :mt],
                                          start=(k == 0), stop=(k == KC - 1)), w=[self.ss], r=[self.ones, sq])
        r = self.rstd
        p.op("dve", lambda e: e.tensor_scalar(out=r[:, :mt], in0=self.ss[:, :mt], scalar1=1.0 / D, scalar2=EPS,
                                              op0=ALU.mult, op1=ALU.add), w=[r], r=[self.ss])
        p.op("act", lambda e: e.activation(out=r[:, :mt], in_=r[:, :mt], func=AF.Sqrt), w=[r], r=[r])
        p.op("dve", lambda e: e.reciprocal(out=r[:, :mt], in_=r[:, :mt]), w=[r], r=[r])

    def apply(self, xs, mt, g, sh, out_fn):
        p = self.p
        for k in range(KC):
            t = self.tmp[k % 2]
            p.op("dve", lambda e: e.scalar_tensor_tensor(out=t[:, :mt], in0=xs[:, k, :mt], scalar=g[:, k:k + 1],
                                                         in1=self.rstd[:, :mt], op0=ALU.mult, op1=ALU.mult),
                 w=[t], r=[xs, g, self.rstd])
            oap, ob = out_fn(k)
            if sh is None:
                p.op("act", lambda e: e.activation(out=oap, in_=t[:, :mt], func=AF.Copy), w=[ob], r=[t])
            else:
                p.op("act", lambda e: e.activation(out=oap, in_=t[:, :mt], func=AF.Identity, bias=sh[:, k:k + 1],
                                                   scale=1.0), w=[ob], r=[t, sh])


def load_vec(p, name):
    d = p.din(name, [128, KC])
    b = p.sb([128, KC], F32, "v_" + name)
    p.dma("sp", b[:, :], d, w=[b])
    return b


def make_gvec(p, nw, sc, name):
    g = p.sb([128, KC], F32, name)
    p.op("dve", lambda e: e.scalar_tensor_tensor(out=g[:, :], in0=sc[:, :], scalar=1.0, in1=nw[:, :],
                                                 op0=ALU.add, op1=ALU.mult), w=[g], r=[sc, nw])
    return g


class WStream:
    def __init__(self, p, kc, nb, nbuf=2, name="ws"):
        self.p = p
        self.bufs = [p.sb([128, kc, nb], BF16, f"{name}{i}") for i in range(nbuf)]
        self.i = 0

    def load(self, wap):
        b = self.bufs[self.i % len(self.bufs)]
        self.i += 1
        n = wap.shape[1]
        self.p.dma("pool", b[:, :, :n], wap.rearrange("(k p) n -> p k n", p=128), w=[b])
        return b


def build_inproj(N, MX=1024, MC=32, NB=256):
    p = Prog()
    M = MX + MC
    xT = p.din("xT", [D, M])
    w = p.din("w", [D, N])
    yT = p.dout("yT", [N, M])
    nw = load_vec(p, "nw")
    scx, shx, scc, shc = [load_vec(p, n) for n in ("scx", "shx", "scc", "shc")]
    gx = make_gvec(p, nw, scx, "gx")
    gc = make_gvec(p, nw, scc, "gc")
    nctx = NormCtx(p)
    xb = p.sb([128, KC, M], BF16, "xb")
    xs = p.sb([128, KC, 512], F32, "xs")
    tiles = [(m0, min(512, MX - m0), gx, shx) for m0 in range(0, MX, 512)] + [(MX, MC, gc, shc)]
    xTv = xT.rearrange("(k p) m -> p k m", p=128)
    for (m0, mt, g, sh) in tiles:
        p.dma("sp", xs[:, :, :mt], xTv[:, :, m0:m0 + mt], w=[xs])
        nctx.stats(xs, mt)
        nctx.apply(xs, mt, g, sh, lambda k: (xb[:, k, m0:m0 + mt], xb))
    ws = WStream(p, KC, NB, 2)
    accs = [p.ps([128, 512], F32, f"acc{i}") for i in range(4)]
    outs = [p.sb([128, M], F32, f"ob{i}") for i in range(3)]
    ai = 0
    oi = 0
    for n0 in range(0, N, NB):
        nb = min(NB, N - n0)
        wt = ws.load(w[:, n0:n0 + nb])
        for c0 in range(0, nb, 128):
            cn = min(128, nb - c0)
            ob = outs[oi % 3]
            oi += 1
            for (m0, mt, _, _) in tiles:
                acc = accs[ai % 4]
                ai += 1
                for k in range(KC):
                    p.op("pe", lambda e: e.matmul(acc[:cn, :mt], lhsT=wt[:, k, c0:c0 + cn], rhs=xb[:, k, m0:m0 + mt],
                                                  start=(k == 0), stop=(k == KC - 1)), w=[acc], r=[wt, xb])
                eng = "act" if (ai % 2) else "dve"
                if eng == "act":
                    p.op("act", lambda e: e.activation(out=ob[:cn, m0:m0 + mt], in_=acc[:cn, :mt], func=AF.Copy),
                         w=[ob], r=[acc])
                else:
                    p.op("dve", lambda e: e.tensor_copy(out=ob[:cn, m0:m0 + mt], in_=acc[:cn, :mt]), w=[ob], r=[acc])
            p.dma("sp", yT[n0 + c0:n0 + c0 + cn, :], ob[:cn, :], r=[ob], is_output=True)
    return p.finish()


NEG = -1.0e30


def ssd_consts():
    s = np.arange(128)[:, None]
    l = np.arange(128)[None, :]
    tri = (s <= l).astype(np.float32)
    triT = (s >= l).astype(np.float32)
    sel127 = np.ones((128, 128), np.float32)
    sel0 = np.ones((128, 128), np.float32)
    maskF = np.where(l >= s, 0.0, NEG).astype(np.float32)
    maskB = np.where(l <= s, 0.0, NEG).astype(np.float32)
    return np.stack([tri, triT, sel127, sel0, maskF, maskB], 0)


def bc3(ap, n):
    return ap.rearrange("p (a o) -> p a o", o=1).to_broadcast([ap.shape[0], ap.shape[1], n])


def build_ssd(TC=256, TX=8192, seq_list=("c", "x"), stage=9):
    p = Prog()
    cst_d = p.din("cst", [6, 128, 128])
    cw_tm_d = p.din("cw_tm", [128, 4, 384]); cb_tm_d = p.din("cb_tm", [128, 384])
    cw_fm_d = p.din("cw_fm", [128, 2, 4]); cb_fm_d = p.din("cb_fm", [128, 2])
    dtb_d = p.din("dtb", [128, 8]); alog_d = p.din("alog", [128, 8]); dsk_d = p.din("dsk", [128, 256])
    seqs = {}
    for nm, T in (("c", TC), ("x", TX)):
        seqs[nm] = dict(T=T, xbtm=p.din(f"xbtm_{nm}", [T + 3, 384]), bcfm=p.din(f"bcfm_{nm}", [256, T + 3]),
                        ztm=p.din(f"ztm_{nm}", [T, 256]), dttm=p.din(f"dttm_{nm}", [128, T // 128, 8]),
                        y=p.dout(f"y_{nm}", [T, 256]))
    cst = p.sb([128, 6, 128], F32, "cst")
    p.dma("sp", cst[:, :, :], cst_d.rearrange("c p l -> p c l"), w=[cst])
    TRI = [cst[:, 0, :], cst[:, 1, :]]; SEL = [cst[:, 2, :], cst[:, 3, :]]; MASK = [cst[:, 4, :], cst[:, 5, :]]

    def ld(name, d, shape):
        b = p.sb(shape, F32, name)
        p.dma("sp", b[tuple(slice(None) for _ in shape)], d, w=[b])
        return b
    cw_tm = ld("cw_tm", cw_tm_d, [128, 4, 384]); cb_tm = ld("cb_tm", cb_tm_d, [128, 384])
    cw_fm = ld("cw_fm", cw_fm_d, [128, 2, 4]); cb_fm = ld("cb_fm", cb_fm_d, [128, 2])
    dtb = ld("dtb", dtb_d, [128, 8]); alog = ld("alog", alog_d, [128, 8]); dsk = ld("dsk", dsk_d, [128, 256])
    ea = p.sb([128, 8], F32, "ea")
    p.op("act", lambda e: e.activation(out=ea[:, :], in_=alog[:, :], func=AF.Exp), w=[ea], r=[alog])
    S32 = [p.sb([128, 256], F32, f"S32_{d}") for d in range(2)]
    Sb = [p.sb([128, 256], BF16, f"Sb_{d}") for d in range(2)]
    for d in range(2):
        p.op("dve", lambda e: e.memset(S32[d][:, :], 0.0), w=[S32[d]])
        p.op("dve", lambda e: e.memset(Sb[d][:, :], 0.0), w=[Sb[d]])
    FT = 1024
    rawfm = p.sb([128, FT + 3], F32, "rawfm"); tmpfm = p.sb([128, FT], F32, "tmpfm")
    rt = [p.sb([128, 4, 384], F32, f"rt{i}") for i in range(2)]
    prod = p.sb([128, 4, 384], F32, "prod"); s1 = p.sb([128, 384], F32, "s1"); s2 = p.sb([128, 384], F32, "s2")
    xsf = p.sb([128, 384], F32, "xsf")
    cbt = [p.sb([128, 128], F32, f"cbt{i}") for i in range(2)]
    xd = [p.sb([128, 256], BF16, f"xd{i}") for i in range(2)]
    xdw = [p.sb([128, 256], BF16, f"xdw{i}") for i in range(2)]
    arg = [p.sb([128, 128], F32, f"arg{i}") for i in range(2)]
    dec = [p.sb([128, 128], F32, f"dec{i}") for i in range(2)]
    mt = [p.sb([128, 128], BF16, f"mt{i}") for i in range(2)]
    yt = p.sb([128, 256], F32, "yt")
    zt = [p.sb([128, 256], F32, f"zt{i}") for i in range(2)]
    ot = [p.sb([128, 256], F32, f"ot{i}") for i in range(2)]
    yl = [p.sb([128, 256], F32, f"yl{i}") for i in range(2)]
    ps_cb = p.ps([128, 128], F32, "ps_cb"); ps_z = p.ps([128, 256], F32, "ps_z")
    ps_row = [p.ps([128, 128], F32, f"ps_row{i}") for i in range(2)]
    ps_yd = p.ps([128, 256], F32, "ps_yd"); ps_ds = p.ps([128, 512], F32, "ps_ds")
    ps_cum = ps_ds

    for nm in seq_list:
        q = seqs[nm]; T = q["T"]; nch = T // 128; n4 = nch * 4
        BT = p.sb([128, T], BF16, f"BT_{nm}"); CT = p.sb([128, T], BF16, f"CT_{nm}")
        xs_tm = p.sb([128, nch, 256], BF16, f"xs_{nm}"); B_tm = p.sb([128, nch, 128], BF16, f"Btm_{nm}")
        ydr = Buf(p, None, f"ydr_{nm}")
        for bi, dst in ((0, BT), (1, CT)):
            for t0 in range(0, T, FT):
                ft = min(FT, T - t0)
                p.dma("sp", rawfm[:, :ft + 3], q["bcfm"][bi * 128:(bi + 1) * 128, t0:t0 + ft + 3], w=[rawfm])
                p.op("dve", lambda e: e.tensor_scalar(out=tmpfm[:, :ft], in0=rawfm[:, 0:ft], scalar1=cw_fm[:, bi, 0:1],
                                                      scalar2=None, op0=ALU.mult), w=[tmpfm], r=[rawfm, cw_fm])
                for j in range(1, 4):
                    p.op("dve", lambda e: e.scalar_tensor_tensor(out=tmpfm[:, :ft], in0=rawfm[:, j:j + ft],
                                                                 scalar=cw_fm[:, bi, j:j + 1], in1=tmpfm[:, :ft],
                                                                 op0=ALU.mult, op1=ALU.add), w=[tmpfm], r=[rawfm, cw_fm])
                p.op("act", lambda e: e.activation(out=dst[:, t0:t0 + ft], in_=tmpfm[:, :ft], func=AF.Silu,
                                                   bias=cb_fm[:, bi:bi + 1], scale=1.0), w=[dst], r=[tmpfm, cb_fm])
        for c in range(nch):
            r_ = rt[c % 2]
            for j in range(4):
                p.dma("sp", r_[:, j, :], q["xbtm"][c * 128 + j:c * 128 + j + 128, :], w=[r_])
            p.op("dve", lambda e: e.tensor_tensor(out=prod[:, :, :], in0=r_[:, :, :], in1=cw_tm[:, :, :], op=ALU.mult),
                 w=[prod], r=[r_, cw_tm])
            p.op("dve", lambda e: e.tensor_tensor(out=s1[:, :], in0=prod[:, 0, :], in1=prod[:, 1, :], op=ALU.add), w=[s1], r=[prod])
            p.op("dve", lambda e: e.tensor_tensor(out=s2[:, :], in0=prod[:, 2, :], in1=prod[:, 3, :], op=ALU.add), w=[s2], r=[prod])
            p.op("dve", lambda e: e.tensor_tensor(out=s1[:, :], in0=s1[:, :], in1=s2[:, :], op=ALU.add), w=[s1], r=[s1, s2])
            p.op("dve", lambda e: e.tensor_tensor(out=s1[:, :], in0=s1[:, :], in1=cb_tm[:, :], op=ALU.add), w=[s1], r=[s1, cb_tm])
            p.op("act", lambda e: e.activation(out=xsf[:, :], in_=s1[:, :], func=AF.Silu), w=[xsf], r=[s1])
            p.op("act", lambda e: e.activation(out=xs_tm[:, c, :], in_=xsf[:, 0:256], func=AF.Copy), w=[xs_tm], r=[xsf])
            p.op("act", lambda e: e.activation(out=B_tm[:, c, :], in_=xsf[:, 256:384], func=AF.Copy), w=[B_tm], r=[xsf])
            o_ = ot[c % 2]
            p.op("dve", lambda e: e.tensor_tensor(out=o_[:, :], in0=xsf[:, 0:256], in1=dsk[:, :], op=ALU.mult),
                 w=[o_], r=[xsf, dsk])
            p.dma("sp", q["y"][c * 128:(c + 1) * 128, :], o_[:, :], w=[ydr], r=[o_], is_output=True)
        if stage < 1:
            continue
        dtr = p.sb([128, nch, 8], F32, f"dtr_{nm}"); t1 = p.sb([128, nch, 8], F32, f"t1_{nm}")
        t2 = p.sb([128, nch, 8], F32, f"t2_{nm}")
        dt2 = p.sb([128, 2, nch, 4], F32, f"dt2_{nm}"); da2 = p.sb([128, 2, nch, 4], F32, f"da2_{nm}")
        cum = p.sb([128, 2, nch, 4], F32, f"cum_{nm}"); wdec = p.sb([128, 2, nch, 4], F32, f"wdec_{nm}")
        etot = p.sb([128, 2, nch, 4], F32, f"etot_{nm}"); ecum = p.sb([128, 2, nch, 4], F32, f"ecum_{nm}")
        p.dma("sp", dtr[:, :, :], q["dttm"], w=[dtr])
        dtb_b = dtb[:, :].rearrange("p (o e) -> p o e", o=1).to_broadcast([128, nch, 8])
        ea_b = ea[:, :].rearrange("p (o e) -> p o e", o=1).to_broadcast([128, nch, 8])
        p.op("dve", lambda e: e.tensor_tensor(out=t1[:, :, :], in0=dtr[:, :, :], in1=dtb_b, op=ALU.add), w=[t1], r=[dtr, dtb])
        p.op("dve", lambda e: e.tensor_scalar(out=t2[:, :, :], in0=t1[:, :, :], scalar1=-1.0, scalar2=None, op0=ALU.mult), w=[t2], r=[t1])
        p.op("dve", lambda e: e.tensor_tensor(out=t2[:, :, :], in0=t2[:, :, :], in1=t1[:, :, :], op=ALU.max), w=[t2], r=[t2, t1])
        p.op("act", lambda e: e.activation(out=t2[:, :, :], in_=t2[:, :, :], func=AF.Exp, scale=-1.0), w=[t2], r=[t2])
        p.op("act", lambda e: e.activation(out=t2[:, :, :], in_=t2[:, :, :], func=AF.Ln, bias=1.0, scale=1.0), w=[t2], r=[t2])
        p.op("dve", lambda e: e.scalar_tensor_tensor(out=t1[:, :, :], in0=t1[:, :, :], scalar=0.0, in1=t2[:, :, :],
                                                     op0=ALU.max, op1=ALU.add), w=[t1], r=[t1, t2])
        if stage < 1.2:
            continue
        for d in range(2):
            p.op("dve", lambda e: e.tensor_copy(out=dt2[:, d, :, :], in_=t1[:, :, d * 4:(d + 1) * 4]), w=[dt2], r=[t1])
            p.op("dve", lambda e: e.scalar_tensor_tensor(out=da2[:, d, :, :], in0=t1[:, :, d * 4:(d + 1) * 4], scalar=-1.0,
                                                         in1=ea_b[:, :, d * 4:(d + 1) * 4], op0=ALU.mult, op1=ALU.mult),
                 w=[da2], r=[t1, ea])
        if stage < 1.4:
            continue
        for d in range(2):
            p.op("pe", lambda e: e.matmul(ps_cum[:, d * n4:(d + 1) * n4], lhsT=TRI[d], rhs=da2[:, d, :, :].rearrange("p c h -> p (c h)"),
                                          start=True, stop=True), w=[ps_cum], r=[cst, da2])
        p.op("dve", lambda e: e.tensor_copy(out=cum[:, :, :, :].rearrange("p d c h -> p (d c h)"), in_=ps_cum[:, :2 * n4]),
             w=[cum], r=[ps_cum])
        if stage < 1.6:
            continue
        for d in range(2):
            pst = (ps_z, ps_yd)[d]
            p.op("pe", lambda e: e.matmul(pst[:, 0:n4], lhsT=SEL[d], rhs=da2[:, d, :, :].rearrange("p c h -> p (c h)"),
                                          start=True, stop=True), w=[pst], r=[cst, da2])
        fl = lambda b: b[:, :, :, :].rearrange("p d c h -> p (d c h)")
        if stage < 1.8:
            continue
        fd = lambda b, d: b[:, d, :, :].rearrange("p c h -> p (c h)")
        for d in range(2):
            pst = (ps_z, ps_yd)[d]
            if stage >= 1.82:
                p.op("act", lambda e: e.activation(out=fd(etot, d), in_=pst[:, :n4], func=AF.Exp), w=[etot], r=[pst])
            if stage >= 1.84:
                p.op("dve", lambda e: e.tensor_tensor(out=fd(wdec, d), in0=pst[:, :n4], in1=fd(cum, d), op=ALU.subtract), w=[wdec], r=[pst, cum])
        if stage >= 1.86:
            p.op("act", lambda e: e.activation(out=fl(wdec), in_=fl(wdec), func=AF.Exp), w=[wdec], r=[wdec])
        if stage >= 1.88:
            p.op("act", lambda e: e.activation(out=fl(ecum), in_=fl(cum), func=AF.Exp), w=[ecum], r=[cum])
        if stage < 2.0:
            continue
        it = 0
        for d in range(2):
            order = range(nch) if d == 0 else range(nch - 1, -1, -1)
            for c in order:
                cs = slice(c * 128, (c + 1) * 128)
                cb_ = cbt[it % 2]; xd_ = xd[it % 2]; xw_ = xdw[it % 2]
                it += 1
                p.op("pe", lambda e: e.matmul(ps_cb[:, :], lhsT=BT[:, cs], rhs=CT[:, cs], start=True, stop=True),
                     w=[ps_cb], r=[BT, CT])
                p.op("act", lambda e: e.activation(out=cb_[:, :], in_=ps_cb[:, :], func=AF.Copy), w=[cb_], r=[ps_cb])
                p.op("dve", lambda e: e.tensor_tensor(out=xd_[:, :].rearrange("p (h q) -> p h q", h=4),
                                                      in0=xs_tm[:, c, :].rearrange("p (h q) -> p h q", h=4),
                                                      in1=bc3(dt2[:, d, c, :], 64), op=ALU.mult), w=[xd_], r=[xs_tm, dt2])
                p.op("pe", lambda e: e.matmul(ps_z[:, :], lhsT=CT[:, cs], rhs=Sb[d][:, :], start=True, stop=True),
                     w=[ps_z], r=[CT, Sb[d]])
                for h in range(4):
                    pr = ps_row[h % 2]; ar = arg[h % 2]; de = dec[h % 2]; m_ = mt[h % 2]
                    p.op("pe", lambda e: e.matmul(pr[:, :], lhsT=da2[:, d, c, h:h + 1].to_broadcast([128, 128]), rhs=TRI[d],
                                                  start=True, stop=True), w=[pr], r=[da2, cst])
                    p.op("dve", lambda e: e.scalar_tensor_tensor(out=ar[:, :], in0=pr[:, :], scalar=cum[:, d, c, h:h + 1],
                                                                 in1=MASK[d], op0=ALU.subtract, op1=ALU.add),
                         w=[ar], r=[pr, cum, cst])
                    p.op("act", lambda e: e.activation(out=de[:, :], in_=ar[:, :], func=AF.Exp), w=[de], r=[ar])
                    p.op("dve", lambda e: e.tensor_tensor(out=m_[:, :], in0=cb_[:, :], in1=de[:, :], op=ALU.mult),
                         w=[m_], r=[cb_, de])
                    p.op("pe", lambda e: e.matmul(ps_yd[:, h * 64:(h + 1) * 64], lhsT=m_[:, :], rhs=xd_[:, h * 64:(h + 1) * 64],
                                                  start=True, stop=True), w=[ps_yd], r=[m_, xd_])
                ya = yl[it % 2]; o_ = ot[it % 2]
                p.dma("sp", ya[:, :], q["y"][c * 128:(c + 1) * 128, :], w=[ya], r=[ydr])
                p.op("dve", lambda e: e.tensor_tensor(out=yt[:, :].rearrange("p (h q) -> p h q", h=4),
                                                      in0=ps_z[:, :].rearrange("p (h q) -> p h q", h=4),
                                                      in1=bc3(ecum[:, d, c, :], 64), op=ALU.mult), w=[yt], r=[ps_z, ecum])
                p.op("dve", lambda e: e.tensor_tensor(out=yt[:, :], in0=ps_yd[:, :], in1=yt[:, :], op=ALU.add),
                     w=[yt], r=[ps_yd, yt])
                if d == 0:
                    p.op("dve", lambda e: e.tensor_tensor(out=o_[:, :], in0=yt[:, :], in1=ya[:, :], op=ALU.add),
                         w=[o_], r=[yt, ya])
                else:
                    z_ = zt[it % 2]
                    p.dma("sp", z_[:, :], q["ztm"][c * 128:(c + 1) * 128, :], w=[z_])
                    p.op("act", lambda e: e.activation(out=z_[:, :], in_=z_[:, :], func=AF.Silu), w=[z_], r=[z_])
                    p.op("dve", lambda e: e.tensor_tensor(out=yt[:, :], in0=yt[:, :], in1=ya[:, :], op=ALU.add),
                         w=[yt], r=[yt, ya])
                    p.op("dve", lambda e: e.tensor_tensor(out=o_[:, :], in0=yt[:, :], in1=z_[:, :], op=ALU.mult),
                         w=[o_], r=[yt, z_])
                p.dma("sp", q["y"][c * 128:(c + 1) * 128, :], o_[:, :], w=[ydr], r=[o_], is_output=True)
                p.op("dve", lambda e: e.tensor_tensor(out=xw_[:, :].rearrange("p (h q) -> p h q", h=4),
                                                      in0=xd_[:, :].rearrange("p (h q) -> p h q", h=4),
                                                      in1=bc3(wdec[:, d, c, :], 64), op=ALU.mult), w=[xw_], r=[xd_, wdec])
                p.op("pe", lambda e: e.matmul(ps_ds[:, :256], lhsT=B_tm[:, c, :], rhs=xw_[:, :], start=True, stop=True),
                     w=[ps_ds], r=[B_tm, xw_])
                p.op("dve", lambda e: e.tensor_tensor(out=S32[d][:, :].rearrange("p (h q) -> p h q", h=4),
                                                      in0=S32[d][:, :].rearrange("p (h q) -> p h q", h=4),
                                                      in1=bc3(etot[:, d, c, :], 64), op=ALU.mult), w=[S32[d]], r=[S32[d], etot])
                p.op("dve", lambda e: e.tensor_tensor(out=S32[d][:, :], in0=S32[d][:, :], in1=ps_ds[:, :256], op=ALU.add),
                     w=[S32[d]], r=[S32[d], ps_ds])
                p.op("act", lambda e: e.activation(out=Sb[d][:, :], in_=S32[d][:, :], func=AF.Copy), w=[Sb[d]], r=[S32[d]])
    return p.finish()


def ssd_inputs(Pc, Px, conv_w, conv_b, dt_bias, a_log, d_skip):
    cst = ssd_consts()
    maps = []
    for i in range(8):
        g = i // 2
        xs_cols = np.arange(2048 + 256 * i, 2048 + 256 * i + 256)
        b_cols = np.arange(4096 + 128 * g, 4096 + 128 * g + 128)
        c_cols = np.arange(4608 + 128 * g, 4608 + 128 * g + 128)
        dt_cols = np.concatenate([5120 + d * 32 + 4 * i + np.arange(4) for d in range(2)])
        ch_tm = np.concatenate([xs_cols, b_cols]) - 2048
        ch_fm = np.concatenate([b_cols, c_cols]) - 2048
        m = {"cst": cst}
        m["cw_tm"] = np.ascontiguousarray(np.broadcast_to(conv_w[:, ch_tm][None], (128, 4, 384)))
        m["cb_tm"] = np.ascontiguousarray(np.broadcast_to(conv_b[ch_tm][None], (128, 384)))
        cwf = conv_w[:, ch_fm]
        m["cw_fm"] = np.ascontiguousarray(cwf.reshape(4, 2, 128).transpose(2, 1, 0))
        m["cb_fm"] = np.ascontiguousarray(conv_b[ch_fm].reshape(2, 128).T)
        hs = 4 * i + np.arange(4)
        m["dtb"] = np.ascontiguousarray(np.broadcast_to(dt_bias[:, hs].reshape(1, 8), (128, 8)))
        m["alog"] = np.ascontiguousarray(np.broadcast_to(a_log[:, hs].reshape(1, 8), (128, 8)))
        m["dsk"] = np.ascontiguousarray(np.broadcast_to(np.repeat(d_skip[hs], 64)[None], (128, 256)))
        for nm, P in (("c", Pc), ("x", Px)):
            T = P.shape[0]
            xb = np.zeros((T + 3, 384), np.float32); xb[2:T + 2] = P[:, np.concatenate([xs_cols, b_cols])]
            bc = np.zeros((256, T + 3), np.float32); bc[:, 2:T + 2] = P[:, np.concatenate([b_cols, c_cols])].T
            m[f"xbtm_{nm}"] = xb; m[f"bcfm_{nm}"] = bc
            m[f"ztm_{nm}"] = np.ascontiguousarray(P[:, 256 * i:256 * i + 256])
            m[f"dttm_{nm}"] = np.ascontiguousarray(P[:, dt_cols].reshape(T // 128, 128, 8).transpose(1, 0, 2))
        maps.append(m)
    return maps


GW = 64
ROWS = 128
DH = 128
SCALE = DH ** -0.5
OFFS = {0: 7, 1: 6, 2: 5, 3: 4, 125: 2, 126: 1, 127: 0}


def row_info(r):
    rs = min(max(r - 4, 0), ROWS - 8)
    return rs, OFFS.get(r, 3)


def na_tables(rpb_h):
    q = np.arange(GW)[:, None]
    k = np.arange(GW)[None, :]
    dcol = np.clip(k - q + 15, 0, 30)
    out = np.zeros((8, GW, 8, GW), np.float32)
    for o in range(8):
        for a in range(8):
            out[o, :, a, :] = rpb_h[a + o][dcol]
    return out.reshape(8, GW, 512)


def na_mask():
    q = np.arange(GW)[:, None]
    k = np.arange(GW)[None, :]
    cstart = np.clip(q - 8, 0, GW - 16)
    ok = (k >= cstart) & (k < cstart + 16)
    m = np.where(ok, 0.0, -30000.0).astype(np.float32)
    return np.ascontiguousarray(np.broadcast_to(m[:, None, :], (GW, 8, GW)).reshape(GW, 512))


def build_na(T=8192, TC=256):
    p = Prog()
    qT_d = p.din("qT", [2, 128, T]); kT_d = p.din("kT", [2, 128, T]); v_d = p.din("v", [2, GW, ROWS, DH])
    qcT_d = p.din("qcT", [2, 128, TC]); kcT_d = p.din("kcT", [2, 128, TC]); vc_d = p.din("vc", [2, 128, TC // 128, DH])
    bias_d = p.din("bias", [2, GW, 8, 512]); mask_d = p.din("mask", [GW, 512]); ident_d = p.din("ident", [128, 128])
    y = p.dout("y", [T, 256]); yc = p.dout("yc", [TC, 256])
    ident32 = p.sb([128, 128], F32, "ident32"); ident = p.sb([128, 128], BF16, "ident")
    p.dma("sp", ident32[:, :], ident_d, w=[ident32])
    p.op("dve", lambda e: e.tensor_copy(out=ident[:, :], in_=ident32[:, :]), w=[ident], r=[ident32])
    mask = p.sb([GW, 512], F32, "mask")
    p.dma("sp", mask[:, :], mask_d, w=[mask])
    qT = p.sb([128, T], BF16, "qT"); kT = p.sb([128, T], BF16, "kT"); v = p.sb([GW, ROWS, DH], BF16, "v")
    qcT = p.sb([128, TC], BF16, "qcT"); kcT = p.sb([128, TC], BF16, "kcT"); vc = p.sb([128, TC // 128, DH], BF16, "vc")
    bias = p.sb([GW, 8, 512], F32, "bias")
    ps_loc = [p.ps([128, 512], F32, f"ps_loc{i}") for i in range(2)]
    ps_ctx = [p.ps([128, 256], F32, "ps_ctx0")] * 2
    ps_t = p.ps([GW, 512], BF16, "ps_t"); ps_tc = p.ps([128, 256], BF16, "ps_tc")
    ps_o = [p.ps([128, DH], F32, f"ps_o{i}") for i in range(2)]
    sc = [p.sb([128, 768], F32, f"sc{i}") for i in range(2)]
    pex = [p.sb([128, 768], BF16, f"pex{i}") for i in range(2)]
    mx = [p.sb([128, 1], F32, f"mx{i}") for i in range(2)]
    ssum = [p.sb([128, 1], F32, f"ssum{i}") for i in range(2)]
    pt = [p.sb([GW, 512], BF16, f"pt{i}") for i in range(2)]
    ptc = [p.sb([128, 256], BF16, f"ptc{i}") for i in range(2)]
    ot = [p.sb([128, DH], F32, f"ot{i}") for i in range(3)]
    it = 0
    for h in range(2):
        p.dma("pool", qT[:, :], qT_d[h], w=[qT]); p.dma("pool", kT[:, :], kT_d[h], w=[kT])
        p.dma("pool", v[:, :, :], v_d[h], w=[v])
        p.dma("pool", qcT[:, :], qcT_d[h], w=[qcT]); p.dma("pool", kcT[:, :], kcT_d[h], w=[kcT])
        p.dma("pool", vc[:, :, :], vc_d[h], w=[vc])
        p.dma("sp", bias[:, :, :], bias_d[h], w=[bias])
        for o in range(8):
            p.op("dve", lambda e: e.tensor_tensor(out=bias[:, o, :], in0=bias[:, o, :], in1=mask[:, :], op=ALU.add),
                 w=[bias], r=[bias, mask])
        for qt in range(TC // 128):
            i2 = it % 2; it += 1
            pc = ps_ctx[i2]; s_ = sc[i2]; pe_ = pex[i2]; m_ = mx[i2]; ss = ssum[i2]; po = ps_o[i2]; tc_ = ptc[i2]; o_ = ot[it % 3]
            p.op("pe", lambda e: e.matmul(pc[:, :TC], lhsT=qcT[:, qt * 128:(qt + 1) * 128], rhs=kcT[:, :], start=True, stop=True),
                 w=[pc], r=[qcT, kcT])
            p.op("act", lambda e: e.activation(out=s_[:, :TC], in_=pc[:, :TC], func=AF.Copy, scale=SCALE), w=[s_], r=[pc])
            p.op("dve", lambda e: e.tensor_reduce(out=m_[:, :], in_=s_[:, :TC], axis=AX.X, op=ALU.max), w=[m_], r=[s_])
            p.op("dve", lambda e: e.tensor_scalar(out=m_[:, :], in0=m_[:, :], scalar1=-1.0, scalar2=None, op0=ALU.mult), w=[m_], r=[m_])
            p.op("act", lambda e: e.activation(out=pe_[:, :TC], in_=s_[:, :TC], func=AF.Exp, bias=m_[:, 0:1], scale=1.0,
                                               accum_out=ss[:, 0:1]), w=[pe_, ss], r=[s_, m_])
            for c in range(TC // 128):
                p.op("pe", lambda e: e.transpose(ps_tc[:, c * 128:(c + 1) * 128], pe_[:, c * 128:(c + 1) * 128], ident[:, :]),
                     w=[ps_tc], r=[pe_, ident])
            p.op("act", lambda e: e.activation(out=tc_[:, :], in_=ps_tc[:, :], func=AF.Copy), w=[tc_], r=[ps_tc])
            for c in range(TC // 128):
                p.op("pe", lambda e: e.matmul(po[:, :], lhsT=tc_[:, c * 128:(c + 1) * 128], rhs=vc[:, c, :],
                                              start=(c == 0), stop=(c == TC // 128 - 1)), w=[po], r=[tc_, vc])
            p.op("dve", lambda e: e.reciprocal(out=ss[:, :], in_=ss[:, :]), w=[ss], r=[ss])
            p.op("act", lambda e: e.activation(out=o_[:, :], in_=po[:, :], func=AF.Copy, scale=ss[:, 0:1]), w=[o_], r=[po, ss])
            p.dma("sp", yc[qt * 128:(qt + 1) * 128, h * DH:(h + 1) * DH], o_[:, :], r=[o_], is_output=True)
        for r in range(ROWS):
            rs, oi = row_info(r)
            i2 = it % 2; it += 1
            pl = ps_loc[i2]; pc = ps_ctx[i2]; s_ = sc[i2]; pe_ = pex[i2]; m_ = mx[i2]; ss = ssum[i2]; po = ps_o[i2]
            t_ = pt[i2]; tc_ = ptc[i2]; o_ = ot[it % 3]
            qs = slice(r * GW, (r + 1) * GW)
            p.op("pe", lambda e: e.matmul(pl[:GW, :], lhsT=qT[:, qs], rhs=kT[:, rs * GW:rs * GW + 512], start=True, stop=True),
                 w=[pl], r=[qT, kT])
            p.op("pe", lambda e: e.matmul(pc[:GW, :TC], lhsT=qT[:, qs], rhs=kcT[:, :], start=True, stop=True),
                 w=[pc], r=[qT, kcT])
            p.op("dve", lambda e: e.scalar_tensor_tensor(out=s_[:GW, :512], in0=pl[:GW, :], scalar=SCALE, in1=bias[:, oi, :],
                                                         op0=ALU.mult, op1=ALU.add), w=[s_], r=[pl, bias])
            p.op("act", lambda e: e.activation(out=s_[:GW, 512:768], in_=pc[:GW, :TC], func=AF.Copy, scale=SCALE), w=[s_], r=[pc])
            p.op("dve", lambda e: e.tensor_reduce(out=m_[:GW, :], in_=s_[:GW, :], axis=AX.X, op=ALU.max), w=[m_], r=[s_])
            p.op("dve", lambda e: e.tensor_scalar(out=m_[:GW, :], in0=m_[:GW, :], scalar1=-1.0, scalar2=None, op0=ALU.mult), w=[m_], r=[m_])
            p.op("act", lambda e: e.activation(out=pe_[:GW, :], in_=s_[:GW, :], func=AF.Exp, bias=m_[:GW, 0:1], scale=1.0,
                                               accum_out=ss[:GW, 0:1]), w=[pe_, ss], r=[s_, m_])
            for a in range(8):
                p.op("pe", lambda e: e.transpose(ps_t[:, a * GW:(a + 1) * GW], pe_[:GW, a * GW:(a + 1) * GW], ident[:GW, :GW]),
                     w=[ps_t], r=[pe_, ident])
            for c in range(TC // 128):
                p.op("pe", lambda e: e.transpose(ps_tc[:, c * GW:(c + 1) * GW], pe_[:GW, 512 + c * 128:512 + (c + 1) * 128],
                                                 ident[:GW, :GW]), w=[ps_tc], r=[pe_, ident])
            p.op("dve", lambda e: e.tensor_copy(out=t_[:, :], in_=ps_t[:, :]), w=[t_], r=[ps_t])
            p.op("act", lambda e: e.activation(out=tc_[:, :128], in_=ps_tc[:, :128], func=AF.Copy), w=[tc_], r=[ps_tc])
            for a in range(8):
                p.op("pe", lambda e: e.matmul(po[:GW, :], lhsT=t_[:, a * GW:(a + 1) * GW], rhs=v[:, rs + a, :],
                                              start=(a == 0), stop=False), w=[po], r=[t_, v])
            for c in range(TC // 128):
                p.op("pe", lambda e: e.matmul(po[:GW, :], lhsT=tc_[:, c * GW:(c + 1) * GW], rhs=vc[:, c, :],
                                              start=False, stop=(c == TC // 128 - 1)), w=[po], r=[tc_, vc])
            p.op("dve", lambda e: e.reciprocal(out=ss[:GW, :], in_=ss[:GW, :]), w=[ss], r=[ss])
            p.op("act", lambda e: e.activation(out=o_[:GW, :], in_=po[:GW, :], func=AF.Copy, scale=ss[:GW, 0:1]), w=[o_], r=[po, ss])
            p.dma("sp", y[r * GW:(r + 1) * GW, h * DH:(h + 1) * DH], o_[:GW, :], r=[o_], is_output=True)
    return p.finish()


def na_inputs(Pc, Px, rpb):
    mask = na_mask()
    ident = np.eye(128, dtype=np.float32)
    maps = []
    T = Px.shape[0]; TC = Pc.shape[0]
    for i in range(8):
        m = {"mask": mask, "ident": ident}
        hs = [2 * i, 2 * i + 1]
        qc = [5184 + h * 128 for h in hs]; kc = [7232 + h * 128 for h in hs]; vcl = [9280 + h * 128 for h in hs]
        m["qT"] = np.stack([np.ascontiguousarray(Px[:, c:c + 128].T) for c in qc])
        m["kT"] = np.stack([np.ascontiguousarray(Px[:, c:c + 128].T) for c in kc])
        m["v"] = np.stack([np.ascontiguousarray(Px[:, c:c + 128].reshape(ROWS, GW, DH).transpose(1, 0, 2)) for c in vcl])
        m["qcT"] = np.stack([np.ascontiguousarray(Pc[:, c:c + 128].T) for c in qc])
        m["kcT"] = np.stack([np.ascontiguousarray(Pc[:, c:c + 128].T) for c in kc])
        m["vc"] = np.stack([np.ascontiguousarray(Pc[:, c:c + 128].reshape(TC // 128, 128, DH).transpose(1, 0, 2)) for c in vcl])
        m["bias"] = np.stack([np.ascontiguousarray(na_tables(rpb[h]).transpose(1, 0, 2)) for h in hs])
        maps.append(m)
    return maps


WFLAT = 11008


class WPool:
    def __init__(self, p, n=2, flat=WFLAT):
        self.p = p
        self.bufs = [p.sb([128, flat], BF16, f"wp{i}") for i in range(n)]
        self.i = 0

    def load(self, wap):
        K, nb = wap.shape
        kc = K // 128
        b = self.bufs[self.i % len(self.bufs)]
        self.i += 1
        view = b[:, :kc * nb].rearrange("p (k n) -> p k n", n=nb)
        self.p.dma("pool", view, wap.rearrange("(k p) n -> p k n", p=128), w=[b])
        return b, view


def build_tail(kind, tiles, M, gn, final, F=11008, NE=8, FE=3584):
    p = Prog()
    mixT = p.din("mixT", [D, M]); xT = p.din("xT", [D, M]); out = p.dout("xo", [D, M])
    pre = (kind == "moe_pre")
    if pre:
        kind = "moe"
        h_out = p.dout("ho", [D, M], BF16); g_out = p.dout("go", [NE, M])
    out_w = p.din("out_w", [D, D])
    if kind == "dense":
        w1 = p.din("w1", [D, F]); w3 = p.din("w3", [D, F]); w2 = p.din("w2", [F, D])
    else:
        rw_d = p.din("rw", [128, KC, NE])
        if not pre:
            w1 = p.din("w1", [NE, D, FE]); w3 = p.din("w3", [NE, D, FE]); w2 = p.din("w2", [NE, FE, D])
        sel_d = p.din("sel", [NE, NE, 128]); ident_d = p.din("ident", [128, 128])
    nwf = load_vec(p, "nwf")
    mods = {}
    for s in ("x", "c"):
        gm = load_vec(p, f"gmsa_{s}"); sc = load_vec(p, f"scm_{s}"); sh = load_vec(p, f"shm_{s}"); gl = load_vec(p, f"gmlp_{s}")
        mods[s] = dict(gmsa=gm, g=make_gvec(p, nwf, sc, f"gf_{s}"), sh=sh, gmlp=gl)
    if gn:
        snw = load_vec(p, "ssd_nw")
    if final:
        fnw = load_vec(p, "fnw")
    nctx = NormCtx(p)
    MT = max(t[1] for t in tiles)
    x1 = p.sb([128, KC, MT], F32, "x1")
    hb = p.sb([128, KC, MT], BF16, "hb")
    FCH = (F // 128) if kind == "dense" else (FE // 128)
    aT = p.sb([128, max(FCH, KC), MT], BF16, "aT")
    mixb = aT
    wp = WPool(p, 2, WFLAT if kind == "dense" else 8192)
    stg = [p.sb([128, MT], F32, f"stg{i}") for i in range(3)]
    accs = [p.ps([128, 512], F32, f"acc{i}") for i in range(4)]
    sil = [p.sb([128, MT], F32, f"sil{i}") for i in range(2)]
    si = [0]; ai = [0]
    if kind == "moe":
        rw = p.sb([128, KC, NE], F32, "rw"); rwg = {s: p.sb([128, KC, NE], F32, f"rwg_{s}") for s in ("x", "c")}
        p.dma("sp", rw[:, :, :], rw_d, w=[rw])
        for s in ("x", "c"):
            p.op("dve", lambda e: e.tensor_tensor(out=rwg[s][:, :, :], in0=rw[:, :, :],
                                                  in1=mods[s]["g"][:, :].rearrange("p (k o) -> p k o", o=1).to_broadcast([128, KC, NE]),
                                                  op=ALU.mult), w=[rwg[s]], r=[rw, mods[s]["g"]])
        sel = p.sb([NE, NE, 128], F32, "sel"); ident = p.sb([128, 128], F32, "identf")
        p.dma("sp", sel[:, :, :], sel_d.rearrange("e k m -> k e m"), w=[sel]); p.dma("sp", ident[:, :], ident_d, w=[ident])
        gT = p.sb([NE, MT], F32, "gT"); gb = p.sb([128, NE, MT], F32, "gb")
        lg = p.sb([128, NE], F32, "lg"); l2 = p.sb([128, NE], F32, "l2"); msk = p.sb([128, NE], F32, "msk")
        m1 = p.sb([128, 1], F32, "m1"); m2 = p.sb([128, 1], F32, "m2"); den = p.sb([128, 1], F32, "den"); rcol = p.sb([128, 1], F32, "rcol")
        ps_l = p.ps([128, 2 * NE], F32, "ps_l"); ps_g = p.ps([128, 512], F32, "ps_g")
        tmp2 = p.sb([128, MT], F32, "tmp2")

    def nstg():
        si[0] += 1
        return stg[si[0] % 3]

    def nacc():
        ai[0] += 1
        return accs[ai[0] % 4]

    xTv = xT.rearrange("(k p) m -> p k m", p=128)
    mixv = mixT.rearrange("(k p) m -> p k m", p=128)
    outv = out.rearrange("(k p) m -> p k m", p=128)

    for (m0, mt, segs) in tiles:
        if gn:
            for g in range(4):
                for j in range(4):
                    k = 4 * g + j
                    s_ = x1
                    p.dma("sp", x1[:, k, :mt], mixv[:, k, m0:m0 + mt], w=[x1])
                for j in range(4):
                    k = 4 * g + j
                    sq = nctx.sq[k % 2]
                    p.op("act", lambda e: e.activation(out=sq[:, :mt], in_=x1[:, k, :mt], func=AF.Square), w=[sq], r=[x1])
                    p.op("pe", lambda e: e.matmul(nctx.ss[:, :mt], lhsT=nctx.ones[:, :], rhs=sq[:, :mt],
                                                  start=(j == 0), stop=(j == 3)), w=[nctx.ss], r=[nctx.ones, sq])
                r_ = nctx.rstd
                p.op("dve", lambda e: e.tensor_scalar(out=r_[:, :mt], in0=nctx.ss[:, :mt], scalar1=1.0 / 512, scalar2=EPS,
                                                      op0=ALU.mult, op1=ALU.add), w=[r_], r=[nctx.ss])
                p.op("act", lambda e: e.activation(out=r_[:, :mt], in_=r_[:, :mt], func=AF.Sqrt), w=[r_], r=[r_])
                p.op("dve", lambda e: e.reciprocal(out=r_[:, :mt], in_=r_[:, :mt]), w=[r_], r=[r_])
                for j in range(4):
                    k = 4 * g + j
                    p.op("dve", lambda e: e.scalar_tensor_tensor(out=mixb[:, k, :mt], in0=x1[:, k, :mt], scalar=snw[:, k:k + 1],
                                                                 in1=r_[:, :mt], op0=ALU.mult, op1=ALU.mult),
                         w=[mixb], r=[x1, snw, r_])
            k0 = 16
        else:
            k0 = 0
        for k in range(k0, KC):
            s_ = nstg()
            p.dma("sp", s_[:, :mt], mixv[:, k, m0:m0 + mt], w=[s_])
            p.op("act", lambda e: e.activation(out=mixb[:, k, :mt], in_=s_[:, :mt], func=AF.Copy), w=[mixb], r=[s_])
        for n0 in range(0, D, 256):
            wb, wv = wp.load(out_w[:, n0:n0 + 256])
            for c0 in range(0, 256, 128):
                n = (n0 + c0) // 128
                acc = nacc()
                for k in range(KC):
                    p.op("pe", lambda e: e.matmul(acc[:, :mt], lhsT=wv[:, k, c0:c0 + 128], rhs=mixb[:, k, :mt],
                                                  start=(k == 0), stop=(k == KC - 1)), w=[acc], r=[wb, mixb])
                s_ = nstg()
                p.dma("sp", s_[:, :mt], xTv[:, n, m0:m0 + mt], w=[s_])
                for (c0_, c1_, sn) in segs:
                    p.op("dve", lambda e: e.scalar_tensor_tensor(out=x1[:, n, c0_:c1_], in0=acc[:, c0_:c1_],
                                                                 scalar=mods[sn]["gmsa"][:, n:n + 1], in1=s_[:, c0_:c1_],
                                                                 op0=ALU.mult, op1=ALU.add), w=[x1], r=[acc, mods[sn]["gmsa"], s_])
        nctx.stats(x1, mt)
        for (c0_, c1_, sn) in segs:
            for k in range(KC):
                t = nctx.tmp[k % 2]
                p.op("dve", lambda e: e.scalar_tensor_tensor(out=t[:, c0_:c1_], in0=x1[:, k, c0_:c1_], scalar=mods[sn]["g"][:, k:k + 1],
                                                             in1=nctx.rstd[:, c0_:c1_], op0=ALU.mult, op1=ALU.mult),
                     w=[t], r=[x1, mods[sn]["g"], nctx.rstd])
                p.op("act", lambda e: e.activation(out=hb[:, k, c0_:c1_], in_=t[:, c0_:c1_], func=AF.Identity,
                                                   bias=mods[sn]["sh"][:, k:k + 1], scale=1.0), w=[hb], r=[t, mods[sn]["sh"]])
        if kind == "dense":
            for f0 in range(0, F, 256):
                fb = min(256, F - f0)
                w1b, w1v = wp.load(w1[:, f0:f0 + fb])
                w3b, w3v = wp.load(w3[:, f0:f0 + fb])
                for c0 in range(0, fb, 128):
                    fc = (f0 + c0) // 128
                    a1 = nacc(); a3 = nacc()
                    for k in range(KC):
                        p.op("pe", lambda e: e.matmul(a1[:, :mt], lhsT=w1v[:, k, c0:c0 + 128], rhs=hb[:, k, :mt],
                                                      start=(k == 0), stop=(k == KC - 1)), w=[a1], r=[w1b, hb])
                    for k in range(KC):
                        p.op("pe", lambda e: e.matmul(a3[:, :mt], lhsT=w3v[:, k, c0:c0 + 128], rhs=hb[:, k, :mt],
                                                      start=(k == 0), stop=(k == KC - 1)), w=[a3], r=[w3b, hb])
                    sl = sil[fc % 2]
                    p.op("act", lambda e: e.activation(out=sl[:, :mt], in_=a1[:, :mt], func=AF.Silu), w=[sl], r=[a1])
                    p.op("dve", lambda e: e.tensor_tensor(out=aT[:, fc, :mt], in0=a3[:, :mt], in1=sl[:, :mt], op=ALU.mult),
                         w=[aT], r=[a3, sl])
            for n in range(KC):
                wb, wv = wp.load(w2[:, n * 128:(n + 1) * 128])
                acc = nacc()
                for k in range(FCH):
                    p.op("pe", lambda e: e.matmul(acc[:, :mt], lhsT=wv[:, k, :], rhs=aT[:, k, :mt],
                                                  start=(k == 0), stop=(k == FCH - 1)), w=[acc], r=[wb, aT])
                for (c0_, c1_, sn) in segs:
                    p.op("dve", lambda e: e.scalar_tensor_tensor(out=x1[:, n, c0_:c1_], in0=acc[:, c0_:c1_],
                                                                 scalar=mods[sn]["gmlp"][:, n:n + 1], in1=x1[:, n, c0_:c1_],
                                                                 op0=ALU.mult, op1=ALU.add), w=[x1], r=[acc, mods[sn]["gmlp"], x1])
        else:
            for t0 in range(0, mt, 128):
                tb = min(128, mt - t0)
                sn = [s for (c0_, c1_, s) in segs if c0_ <= t0 < c1_][0]
                for k in range(KC):
                    p.op("pe", lambda e: e.matmul(ps_l[:tb, 0:NE], lhsT=x1[:, k, t0:t0 + tb], rhs=rwg[sn][:, k, :],
                                                  start=(k == 0), stop=(k == KC - 1)), w=[ps_l], r=[x1, rwg[sn]])
                for k in range(KC):
                    p.op("pe", lambda e: e.matmul(ps_l[:tb, NE:2 * NE], lhsT=mods[sn]["sh"][:, k:k + 1].to_broadcast([128, tb]),
                                                  rhs=rw[:, k, :], start=(k == 0), stop=(k == KC - 1)), w=[ps_l], r=[mods[sn]["sh"], rw])
                p.op("pe", lambda e: e.matmul(ps_g[:tb, 0:1], lhsT=nctx.rstd[0:1, t0:t0 + tb], rhs=nctx.ones[0:1, 0:1],
                                              start=True, stop=True), w=[ps_g], r=[nctx.rstd, nctx.ones])
                p.op("act", lambda e: e.activation(out=rcol[:tb, :], in_=ps_g[:tb, 0:1], func=AF.Copy), w=[rcol], r=[ps_g])
                p.op("act", lambda e: e.activation(out=l2[:tb, :], in_=ps_l[:tb, NE:2 * NE], func=AF.Copy), w=[l2], r=[ps_l])
                p.op("dve", lambda e: e.scalar_tensor_tensor(out=lg[:tb, :], in0=ps_l[:tb, 0:NE], scalar=rcol[:tb, 0:1], in1=l2[:tb, :],
                                                             op0=ALU.mult, op1=ALU.add), w=[lg], r=[ps_l, rcol, l2])
                p.op("dve", lambda e: e.tensor_reduce(out=m1[:tb, :], in_=lg[:tb, :], axis=AX.X, op=ALU.max), w=[m1], r=[lg])
                p.op("dve", lambda e: e.tensor_scalar(out=msk[:tb, :], in0=lg[:tb, :], scalar1=m1[:tb, 0:1], scalar2=None, op0=ALU.is_equal),
                     w=[msk], r=[lg, m1])
                p.op("dve", lambda e: e.scalar_tensor_tensor(out=l2[:tb, :], in0=msk[:tb, :], scalar=-1.0e30, in1=lg[:tb, :],
                                                             op0=ALU.mult, op1=ALU.add), w=[l2], r=[msk, lg])
                p.op("dve", lambda e: e.tensor_reduce(out=m2[:tb, :], in_=l2[:tb, :], axis=AX.X, op=ALU.max), w=[m2], r=[l2])
                p.op("dve", lambda e: e.tensor_scalar(out=msk[:tb, :], in0=lg[:tb, :], scalar1=m2[:tb, 0:1], scalar2=None, op0=ALU.is_ge),
                     w=[msk], r=[lg, m2])
                p.op("dve", lambda e: e.tensor_scalar(out=m1[:tb, :], in0=m1[:tb, :], scalar1=-1.0, scalar2=None, op0=ALU.mult), w=[m1], r=[m1])
                p.op("act", lambda e: e.activation(out=l2[:tb, :], in_=lg[:tb, :], func=AF.Exp, bias=m1[:tb, 0:1], scale=1.0), w=[l2], r=[lg, m1])
                p.op("dve", lambda e: e.tensor_tensor(out=l2[:tb, :], in0=l2[:tb, :], in1=msk[:tb, :], op=ALU.mult), w=[l2], r=[l2, msk])
                p.op("dve", lambda e: e.tensor_reduce(out=den[:tb, :], in_=l2[:tb, :], axis=AX.X, op=ALU.add), w=[den], r=[l2])
                p.op("dve", lambda e: e.reciprocal(out=den[:tb, :], in_=den[:tb, :]), w=[den], r=[den])
                p.op("dve", lambda e: e.tensor_scalar(out=l2[:tb, :], in0=l2[:tb, :], scalar1=den[:tb, 0:1], scalar2=None, op0=ALU.mult),
                     w=[l2], r=[l2, den])
                p.op("pe", lambda e: e.transpose(ps_g[:NE, 128:128 + tb], l2[:tb, :], ident[:tb, :tb]), w=[ps_g], r=[l2, ident])
                p.op("act", lambda e: e.activation(out=gT[:, t0:t0 + tb], in_=ps_g[:NE, 128:128 + tb], func=AF.Copy), w=[gT], r=[ps_g])
            if pre:
                p.dma("sp", g_out[:, m0:m0 + mt], gT[:, :mt], r=[gT], is_output=True)
                hov = h_out.rearrange("(k p) m -> p k m", p=128)
                p.dma("sp", hov[:, :, m0:m0 + mt], hb[:, :, :mt], r=[hb], is_output=True)
            for ex in range(0 if pre else NE):
                p.op("pe", lambda e: e.matmul(ps_g[:, :mt], lhsT=sel[:, ex, :], rhs=gT[:, :mt], start=True, stop=True),
                     w=[ps_g], r=[sel, gT])
                p.op("act", lambda e: e.activation(out=gb[:, ex, :mt], in_=ps_g[:, :mt], func=AF.Copy), w=[gb], r=[ps_g])
            for ex in range(0 if pre else NE):
                for f0 in range(0, FE, 256):
                    fb = min(256, FE - f0)
                    w1b, w1v = wp.load(w1[ex, :, f0:f0 + fb])
                    w3b, w3v = wp.load(w3[ex, :, f0:f0 + fb])
                    for c0 in range(0, fb, 128):
                        fc = (f0 + c0) // 128
                        a1 = nacc(); a3 = nacc()
                        for k in range(KC):
                            p.op("pe", lambda e: e.matmul(a1[:, :mt], lhsT=w1v[:, k, c0:c0 + 128], rhs=hb[:, k, :mt],
                                                          start=(k == 0), stop=(k == KC - 1)), w=[a1], r=[w1b, hb])
                        for k in range(KC):
                            p.op("pe", lambda e: e.matmul(a3[:, :mt], lhsT=w3v[:, k, c0:c0 + 128], rhs=hb[:, k, :mt],
                                                          start=(k == 0), stop=(k == KC - 1)), w=[a3], r=[w3b, hb])
                        sl = sil[fc % 2]
                        p.op("act", lambda e: e.activation(out=sl[:, :mt], in_=a1[:, :mt], func=AF.Silu), w=[sl], r=[a1])
                        p.op("dve", lambda e: e.tensor_tensor(out=tmp2[:, :mt], in0=a3[:, :mt], in1=sl[:, :mt], op=ALU.mult),
                             w=[tmp2], r=[a3, sl])
                        p.op("pool", lambda e: e.tensor_tensor(out=aT[:, fc, :mt], in0=tmp2[:, :mt], in1=gb[:, ex, :mt], op=ALU.mult),
                             w=[aT], r=[tmp2, gb])
                for n in range(KC):
                    wb, wv = wp.load(w2[ex, :, n * 128:(n + 1) * 128])
                    acc = nacc()
                    for k in range(FCH):
                        p.op("pe", lambda e: e.matmul(acc[:, :mt], lhsT=wv[:, k, :], rhs=aT[:, k, :mt],
                                                      start=(k == 0), stop=(k == FCH - 1)), w=[acc], r=[wb, aT])
                    for (c0_, c1_, sn) in segs:
                        p.op("dve", lambda e: e.scalar_tensor_tensor(out=x1[:, n, c0_:c1_], in0=acc[:, c0_:c1_],
                                                                     scalar=mods[sn]["gmlp"][:, n:n + 1], in1=x1[:, n, c0_:c1_],
                                                                     op0=ALU.mult, op1=ALU.add), w=[x1], r=[acc, mods[sn]["gmlp"], x1])
        if final:
            nctx.stats(x1, mt)
            for k in range(KC):
                s_ = nstg()
                p.op("dve", lambda e: e.scalar_tensor_tensor(out=s_[:, :mt], in0=x1[:, k, :mt], scalar=fnw[:, k:k + 1],
                                                             in1=nctx.rstd[:, :mt], op0=ALU.mult, op1=ALU.mult),
                     w=[s_], r=[x1, fnw, nctx.rstd])
                p.dma("sp", outv[:, k, m0:m0 + mt], s_[:, :mt], r=[s_], is_output=True)
        else:
            for k in range(KC):
                p.dma("sp", outv[:, k, m0:m0 + mt], x1[:, k, :mt], r=[x1], is_output=True)
    return p.finish()


def build_expert(T=8192, FE=3584, MT=1024):
    p = Prog()
    hT = p.din("hT", [D, T], BF16); g_d = p.din("g", [128, T])
    w1 = p.din("w1", [D, FE]); w3 = p.din("w3", [D, FE]); w2 = p.din("w2", [FE, D])
    out = p.dout("yT", [D, T])
    FCH = FE // 128
    SUBS = [(s0, min(512, MT - s0)) for s0 in range(0, MT, 512)]
    hb = p.sb([128, KC, MT], BF16, "hb")
    aT = p.sb([128, FCH, MT], BF16, "aT")
    gb = p.sb([128, MT], F32, "gb")
    wp = WPool(p, 4, 8192)
    accs = [p.ps([128, 512], F32, f"acc{i}") for i in range(4)]
    sil = [p.sb([128, 512], F32, f"sil{i}") for i in range(2)]
    tmp2 = [p.sb([128, 512], F32, f"tmp2{i}") for i in range(2)]
    ot = [p.sb([128, 512], F32, f"ot{i}") for i in range(3)]
    ai = [0]; oi = [0]; ei = [0]

    def nacc():
        ai[0] += 1
        return accs[ai[0] % 4]
    hv = hT.rearrange("(k p) m -> p k m", p=128)
    for ti, m0 in enumerate(range(0, T, MT)):
        p.dma("sp", hb[:, :, :], hv[:, :, m0:m0 + MT], w=[hb])
        p.dma("sp", gb[:, :], g_d[:, m0:m0 + MT], w=[gb])
        for f0 in range(0, FE, 256):
            w1b, w1v = wp.load(w1[:, f0:f0 + 256])
            w3b, w3v = wp.load(w3[:, f0:f0 + 256])
            for c0 in range(0, 256, 128):
                fc = (f0 + c0) // 128
                for (s0, sn) in SUBS:
                    a1 = nacc(); a3 = nacc()
                    for k in range(KC):
                        p.op("pe", lambda e: e.matmul(a1[:, :sn], lhsT=w1v[:, k, c0:c0 + 128], rhs=hb[:, k, s0:s0 + sn],
                                                      start=(k == 0), stop=(k == KC - 1)), w=[a1], r=[w1b, hb])
                    for k in range(KC):
                        p.op("pe", lambda e: e.matmul(a3[:, :sn], lhsT=w3v[:, k, c0:c0 + 128], rhs=hb[:, k, s0:s0 + sn],
                                                      start=(k == 0), stop=(k == KC - 1)), w=[a3], r=[w3b, hb])
                    ei[0] += 1
                    sl = sil[ei[0] % 2]; t2 = tmp2[ei[0] % 2]
                    p.op("act", lambda e: e.activation(out=sl[:, :sn], in_=a1[:, :sn], func=AF.Silu), w=[sl], r=[a1])
                    p.op("dve", lambda e: e.tensor_tensor(out=t2[:, :sn], in0=a3[:, :sn], in1=sl[:, :sn], op=ALU.mult), w=[t2], r=[a3, sl])
                    p.op("dve", lambda e: e.tensor_tensor(out=aT[:, fc, s0:s0 + sn], in0=t2[:, :sn], in1=gb[:, s0:s0 + sn], op=ALU.mult),
                         w=[aT], r=[t2, gb])
        for n0 in range(0, D, 256):
            wb, wv = wp.load(w2[:, n0:n0 + 256])
            for c0 in range(0, 256, 128):
                n = (n0 + c0) // 128
                for (s0, sn) in SUBS:
                    acc = nacc()
                    for k in range(FCH):
                        p.op("pe", lambda e: e.matmul(acc[:, :sn], lhsT=wv[:, k, c0:c0 + 128], rhs=aT[:, k, s0:s0 + sn],
                                                      start=(k == 0), stop=(k == FCH - 1)), w=[acc], r=[wb, aT])
                    oi[0] += 1
                    o_ = ot[oi[0] % 3]
                    if oi[0] % 2:
                        p.op("act", lambda e: e.activation(out=o_[:, :sn], in_=acc[:, :sn], func=AF.Copy), w=[o_], r=[acc])
                    else:
                        p.op("dve", lambda e: e.tensor_copy(out=o_[:, :sn], in_=acc[:, :sn]), w=[o_], r=[acc])
                    p.dma("act", out[n * 128:(n + 1) * 128, m0 + s0:m0 + s0 + sn], o_[:, :sn], r=[o_], is_output=True)
    return p.finish()


def build_combine(M=1024, NE=8, MT=256):
    p = Prog()
    parts = p.din("parts", [NE, D, M]); x1T = p.din("x1T", [D, M]); out = p.dout("xo", [D, M])
    gmlp = load_vec(p, "gmlp"); fnw = load_vec(p, "fnw")
    nctx = NormCtx(p)
    x2 = p.sb([128, KC, MT], F32, "x2")
    pt = [p.sb([128, NE, MT], F32, f"pt{i}") for i in range(2)]
    xs = [p.sb([128, MT], F32, f"xs{i}") for i in range(2)]
    sm = [p.sb([128, MT], F32, f"sm{i}") for i in range(2)]
    ot = [p.sb([128, MT], F32, f"ot{i}") for i in range(3)]
    pv = parts.rearrange("e (k p) m -> k p e m", p=128)
    xv = x1T.rearrange("(k p) m -> p k m", p=128); ov = out.rearrange("(k p) m -> p k m", p=128)
    for m0 in range(0, M, MT):
        for k in range(KC):
            pt_ = pt[k % 2]; xs_ = xs[k % 2]; sm_ = sm[k % 2]
            p.dma("sp", pt_[:, :, :], pv[k, :, :, m0:m0 + MT], w=[pt_])
            p.dma("sp", xs_[:, :], xv[:, k, m0:m0 + MT], w=[xs_])
            p.op("dve", lambda e: e.tensor_tensor(out=pt_[:, 0:4, :], in0=pt_[:, 0:4, :], in1=pt_[:, 4:8, :], op=ALU.add), w=[pt_], r=[pt_])
            p.op("dve", lambda e: e.tensor_tensor(out=pt_[:, 0:2, :], in0=pt_[:, 0:2, :], in1=pt_[:, 2:4, :], op=ALU.add), w=[pt_], r=[pt_])
            p.op("dve", lambda e: e.tensor_tensor(out=sm_[:, :], in0=pt_[:, 0, :], in1=pt_[:, 1, :], op=ALU.add), w=[sm_], r=[pt_])
            p.op("dve", lambda e: e.scalar_tensor_tensor(out=x2[:, k, :], in0=sm_[:, :], scalar=gmlp[:, k:k + 1], in1=xs_[:, :],
                                                         op0=ALU.mult, op1=ALU.add), w=[x2], r=[sm_, gmlp, xs_])
        nctx.stats(x2, MT)
        for k in range(KC):
            o_ = ot[k % 3]
            p.op("dve", lambda e: e.scalar_tensor_tensor(out=o_[:, :], in0=x2[:, k, :], scalar=fnw[:, k:k + 1], in1=nctx.rstd[:, :MT],
                                                         op0=ALU.mult, op1=ALU.mult), w=[o_], r=[x2, fnw, nctx.rstd])
            p.dma("sp", ov[:, k, m0:m0 + MT], o_[:, :], r=[o_], is_output=True)
    return p.finish()


def build_rg(T=8192, TC=256, SC=2048):
    p = Prog()
    rec_d = {"c": p.din("recT_c", [256, TC + 3]), "x": p.din("recT_x", [256, T + 3])}
    gate_d = p.din("gateT", [256, T])
    cw_d = p.din("cw", [128, 2, 4]); cb_d = p.din("cb", [128, 2])
    gw_d = p.din("gw", [128, 2, 2, 2, 128]); gb_d = p.din("gb", [128, 2, 2, 2]); lam_d = p.din("lam", [128, 2, 2])
    y = p.dout("y", [256, T])

    def ld(name, d, shape):
        b = p.sb(shape, F32, name)
        p.dma("sp", b[tuple(slice(None) for _ in shape)], d, w=[b])
        return b
    cw = ld("cw", cw_d, [128, 2, 4]); cb = ld("cb", cb_d, [128, 2]); gw = ld("gw", gw_d, [128, 2, 2, 2, 128])
    gb = ld("gb", gb_d, [128, 2, 2, 2]); lam = ld("lam", lam_d, [128, 2, 2])
    c8 = p.sb([128, 2, 2], F32, "c8"); c16 = p.sb([128, 2, 2], F32, "c16")
    p.op("act", lambda e: e.activation(out=c8[:, :, :], in_=lam[:, :, :], func=AF.Exp, scale=-1.0), w=[c8], r=[lam])
    p.op("act", lambda e: e.activation(out=c8[:, :, :], in_=c8[:, :, :], func=AF.Ln, bias=1.0, scale=1.0), w=[c8], r=[c8])
    p.op("dve", lambda e: e.tensor_scalar(out=c16[:, :, :], in0=c8[:, :, :], scalar1=-16.0, scalar2=None, op0=ALU.mult), w=[c16], r=[c8])
    p.op("dve", lambda e: e.tensor_scalar(out=c8[:, :, :], in0=c8[:, :, :], scalar1=-8.0, scalar2=None, op0=ALU.mult), w=[c8], r=[c8])
    zero = p.sb([128, 1], F32, "zero")
    p.op("dve", lambda e: e.memset(zero[:, :], 0.0), w=[zero])
    U = p.sb([128, T + 3], F32, "U"); A = p.sb([128, T], F32, "A"); INP = p.sb([128, T], F32, "INP"); HF = p.sb([128, T], F32, "HF")
    HB = U
    hst = [[p.sb([128, 1], F32, f"hst{b}{d}") for d in range(2)] for b in range(2)]
    rr = [p.sb([128, 512], F32, f"rr{i}") for i in range(2)]
    ii = [p.sb([128, 512], F32, f"ii{i}") for i in range(2)]
    sq = [p.sb([128, 512], F32, f"sq{i}") for i in range(2)]
    ps_r = [p.ps([128, 512], F32, f"ps_r{i}") for i in range(2)]
    ps_i = [p.ps([128, 512], F32, f"ps_i{i}") for i in range(2)]
    GT = 1024
    gt = [p.sb([128, GT], F32, f"gt{i}") for i in range(2)]
    t1 = [p.sb([128, GT], F32, f"t1{i}") for i in range(2)]
    it = 0
    for b in range(2):
        for nm, TT in (("c", TC), ("x", T)):
            RAW = INP
            CCH = 4096 if T >= 8192 else T // 2
            for t0 in range(0, TT, CCH):
                tt = min(CCH, TT - t0)
                p.dma("sp", A[:, :tt + 3], rec_d[nm][b * 128:(b + 1) * 128, t0:t0 + tt + 3], w=[A])
                p.op("dve", lambda e: e.tensor_scalar(out=U[:, t0:t0 + tt], in0=A[:, 0:tt], scalar1=cw[:, b, 0:1], scalar2=cb[:, b:b + 1],
                                                      op0=ALU.mult, op1=ALU.add), w=[U], r=[A, cw, cb])
                for j in range(1, 4):
                    p.op("dve", lambda e: e.scalar_tensor_tensor(out=U[:, t0:t0 + tt], in0=A[:, j:j + tt], scalar=cw[:, b, j:j + 1],
                                                                 in1=U[:, t0:t0 + tt], op0=ALU.mult, op1=ALU.add), w=[U], r=[A, cw, U])
            for d in range(2):
                for t0 in range(0, TT, 512):
                    tt = min(512, TT - t0)
                    i2 = it % 2; it += 1
                    pr = ps_r[i2]; pi = ps_i[i2]; r_ = rr[i2]; i_ = ii[i2]; s_ = sq[i2]
                    p.op("pe", lambda e: e.matmul(pr[:, :tt], lhsT=gw[:, b, d, 0, :], rhs=U[:, t0:t0 + tt], start=True, stop=True),
                         w=[pr], r=[gw, U])
                    p.op("pe", lambda e: e.matmul(pi[:, :tt], lhsT=gw[:, b, d, 1, :], rhs=U[:, t0:t0 + tt], start=True, stop=True),
                         w=[pi], r=[gw, U])
                    p.op("act", lambda e: e.activation(out=r_[:, :tt], in_=pr[:, :tt], func=AF.Sigmoid, bias=gb[:, b, d, 0:1], scale=1.0),
                         w=[r_], r=[pr, gb])
                    p.op("act", lambda e: e.activation(out=i_[:, :tt], in_=pi[:, :tt], func=AF.Sigmoid, bias=gb[:, b, d, 1:2], scale=1.0),
                         w=[i_], r=[pi, gb])
                    p.op("act", lambda e: e.activation(out=A[:, t0:t0 + tt], in_=r_[:, :tt], func=AF.Exp, scale=c8[:, b, d:d + 1]),
                         w=[A], r=[r_, c8])
                    p.op("act", lambda e: e.activation(out=s_[:, :tt], in_=r_[:, :tt], func=AF.Exp, scale=c16[:, b, d:d + 1]),
                         w=[s_], r=[r_, c16])
                    p.op("dve", lambda e: e.tensor_scalar(out=s_[:, :tt], in0=s_[:, :tt], scalar1=-1.0, scalar2=1.0, op0=ALU.mult, op1=ALU.add),
                         w=[s_], r=[s_])
                    p.op("act", lambda e: e.activation(out=s_[:, :tt], in_=s_[:, :tt], func=AF.Sqrt), w=[s_], r=[s_])
                    p.op("dve", lambda e: e.tensor_tensor(out=i_[:, :tt], in0=i_[:, :tt], in1=U[:, t0:t0 + tt], op=ALU.mult), w=[i_], r=[i_, U])
                    p.op("dve", lambda e: e.tensor_tensor(out=INP[:, t0:t0 + tt], in0=i_[:, :tt], in1=s_[:, :tt], op=ALU.mult),
                         w=[INP], r=[i_, s_])
                H = HF if d == 0 else HB
                init = zero if nm == "c" else hst[b][d]
                nsc = (TT + SC - 1) // SC
                for sidx in range(nsc):
                    if d == 0:
                        lo = sidx * SC; hi = min(TT, lo + SC)
                        ini = init[:, 0:1] if sidx == 0 else H[:, lo - 1:lo]
                        p.op("dve", lambda e: e.tensor_tensor_scan(out=H[:, lo:hi], data0=A[:, lo:hi], data1=INP[:, lo:hi], initial=ini,
                                                                   op0=ALU.mult, op1=ALU.add), w=[H], r=[A, INP, init, H])
                    else:
                        hi = TT - sidx * SC; lo = max(0, hi - SC)
                        ini = init[:, 0:1] if sidx == 0 else H[:, hi:hi + 1]
                        rH = H[:, lo:hi][:, ::-1]; rA = A[:, lo:hi][:, ::-1]; rI = INP[:, lo:hi][:, ::-1]
                        p.op("dve", lambda e: e.tensor_tensor_scan(out=rH, data0=rA, data1=rI, initial=ini,
                                                                   op0=ALU.mult, op1=ALU.add), w=[H], r=[A, INP, init, H])
                if nm == "c":
                    src = H[:, TT - 1:TT] if d == 0 else H[:, 0:1]
                    p.op("dve", lambda e: e.tensor_copy(out=hst[b][d][:, :], in_=src), w=[hst[b][d]], r=[H])
            if nm == "x":
                for t0 in range(0, T, GT):
                    g_ = gt[it % 2]; t_ = t1[it % 2]; it += 1
                    p.dma("sp", g_[:, :], gate_d[b * 128:(b + 1) * 128, t0:t0 + GT], w=[g_])
                    p.op("dve", lambda e: e.tensor_tensor(out=t_[:, :], in0=g_[:, :], in1=g_[:, :], op=ALU.mult), w=[t_], r=[g_])
                    p.op("dve", lambda e: e.tensor_scalar(out=t_[:, :], in0=t_[:, :], scalar1=0.044715, scalar2=1.0, op0=ALU.mult, op1=ALU.add),
                         w=[t_], r=[t_])
                    p.op("dve", lambda e: e.tensor_tensor(out=t_[:, :], in0=t_[:, :], in1=g_[:, :], op=ALU.mult), w=[t_], r=[t_, g_])
                    p.op("act", lambda e: e.activation(out=t_[:, :], in_=t_[:, :], func=AF.Tanh, scale=0.7978845608028654), w=[t_], r=[t_])
                    p.op("dve", lambda e: e.tensor_scalar(out=t_[:, :], in0=t_[:, :], scalar1=0.5, scalar2=0.5, op0=ALU.mult, op1=ALU.add),
                         w=[t_], r=[t_])
                    p.op("dve", lambda e: e.tensor_tensor(out=t_[:, :], in0=t_[:, :], in1=g_[:, :], op=ALU.mult), w=[t_], r=[t_, g_])
                    p.op("dve", lambda e: e.tensor_tensor(out=g_[:, :], in0=HF[:, t0:t0 + GT], in1=HB[:, t0:t0 + GT], op=ALU.add),
                         w=[g_], r=[HF, HB])
                    p.op("dve", lambda e: e.tensor_tensor(out=t_[:, :], in0=t_[:, :], in1=g_[:, :], op=ALU.mult), w=[t_], r=[t_, g_])
                    p.dma("sp", y[b * 128:(b + 1) * 128, t0:t0 + GT], t_[:, :], r=[t_], is_output=True)
    return p.finish()


def rg_inputs(recc, P1, conv_w, conv_b, gate_w, gate_b, lam):
    maps = []
    T = P1.shape[0]; TC = recc.shape[0]
    for i in range(8):
        ch = slice(256 * i, 256 * i + 256)
        m = {}
        rx = np.zeros((256, T + 3), np.float32); rx[:, 2:T + 2] = P1[:, 2048 + 256 * i:2048 + 256 * i + 256].T
        rc = np.zeros((256, TC + 3), np.float32); rc[:, 2:TC + 2] = recc[:, ch].T
        m["recT_x"] = rx; m["recT_c"] = rc
        m["gateT"] = np.ascontiguousarray(P1[:, 256 * i:256 * i + 256].T)
        m["cw"] = np.ascontiguousarray(conv_w[:, ch].reshape(4, 2, 128).transpose(2, 1, 0))
        m["cb"] = np.ascontiguousarray(conv_b[ch].reshape(2, 128).T)
        gwb = gate_w[:, :, 2 * i:2 * i + 2]
        m["gw"] = np.ascontiguousarray(gwb.transpose(3, 2, 0, 1, 4))
        gbb = gate_b[:, :, 2 * i:2 * i + 2]
        m["gb"] = np.ascontiguousarray(gbb.transpose(3, 2, 0, 1))
        m["lam"] = np.ascontiguousarray(lam[:, ch].reshape(2, 2, 128).transpose(2, 1, 0))
        maps.append(m)
    return maps


L = 8192
HY_EMB = 33
HY_FFN = 64
PI = math.pi


def hy_feats():
    f32 = np.float32
    t = np.linspace(0.0, 1.0, L, dtype=f32)[:, None]
    bands = (HY_EMB - 1) // 2
    ang = (f32(2.0 * math.pi / L) * np.arange(L, dtype=f32)[:, None] * np.linspace(1e-4, bands - 1, bands, dtype=f32)[None])
    feats = np.concatenate([t, np.cos(ang), -np.sin(ang)], axis=-1).astype(f32)
    return np.ascontiguousarray(feats.T), np.ascontiguousarray(np.broadcast_to(t[:, 0][None], (128, L)))


def build_hyfilt(NR=1024):
    p = Prog()
    featsT_d = p.din("featsT", [HY_EMB, L]); tlin_d = p.din("tlin", [128, L])
    w_in_d = p.din("w_in", [HY_EMB, HY_FFN]); w_mid_d = p.din("w_mid", [2, HY_FFN, HY_FFN]); w_out_d = p.din("w_out", [HY_FFN, NR])
    vec_d = p.din("vec", [HY_FFN, 4])
    dl_d = p.din("deltas", [128, NR // 128])
    out = p.dout("filt", [NR, L])
    w_in = p.sb([HY_EMB, HY_FFN], F32, "w_in"); w_mid = p.sb([HY_FFN, 2, HY_FFN], F32, "w_mid"); w_out = p.sb([HY_FFN, NR], F32, "w_out")
    vec = p.sb([HY_FFN, 4], F32, "vec"); dl = p.sb([128, NR // 128], F32, "dl"); dln = p.sb([128, NR // 128], F32, "dln")
    p.dma("sp", w_in[:, :], w_in_d, w=[w_in]); p.dma("sp", w_mid[:, :, :], w_mid_d.rearrange("m k f -> k m f"), w=[w_mid])
    p.dma("sp", w_out[:, :], w_out_d, w=[w_out]); p.dma("sp", vec[:, :], vec_d, w=[vec]); p.dma("sp", dl[:, :], dl_d, w=[dl])
    fb = p.sb([HY_FFN, 3], F32, "fb")
    p.op("dve", lambda e: e.tensor_scalar(out=fb[:, :], in0=vec[:, 1:4], scalar1=vec[:, 0:1], scalar2=None, op0=ALU.mult), w=[fb], r=[vec])
    p.op("dve", lambda e: e.tensor_scalar(out=dln[:, :], in0=dl[:, :], scalar1=-1.0, scalar2=None, op0=ALU.mult), w=[dln], r=[dl])
    p.op("dve", lambda e: e.tensor_tensor(out=dln[:, :], in0=dln[:, :], in1=dl[:, :], op=ALU.min), w=[dln], r=[dln, dl])
    fs = p.sb([HY_FFN, 1], F32, "fs"); fbs = p.sb([HY_FFN, 3], F32, "fbs"); mpi = p.sb([HY_FFN, 1], F32, "mpi")
    p.op("dve", lambda e: e.tensor_scalar(out=fs[:, :], in0=vec[:, 0:1], scalar1=1.0 / (2.0 * PI), scalar2=None, op0=ALU.mult), w=[fs], r=[vec])
    p.op("dve", lambda e: e.tensor_scalar(out=fbs[:, :], in0=fb[:, :], scalar1=1.0 / (2.0 * PI), scalar2=4.5, op0=ALU.mult, op1=ALU.add), w=[fbs], r=[fb])
    p.op("dve", lambda e: e.memset(mpi[:, :], -PI), w=[mpi])
    TT = 512
    ki = p.sb([HY_FFN, TT], mybir.dt.int32, "ki"); kf = p.sb([HY_FFN, TT], F32, "kf")
    ft = [p.sb([HY_EMB, TT], F32, f"ft{i}") for i in range(2)]
    tl = [p.sb([128, TT], F32, f"tl{i}") for i in range(2)]
    hid = [p.sb([HY_FFN, TT], F32, f"hid{i}") for i in range(2)]
    ps_h = [p.ps([HY_FFN, TT], F32, f"ps_h{i}") for i in range(2)]
    ps_o = [p.ps([128, TT], F32, f"ps_o{i}") for i in range(2)]
    dec = [p.sb([128, TT], F32, f"dec{i}") for i in range(2)]
    ot = [p.sb([128, TT], F32, f"ot{i}") for i in range(3)]
    oi = 0
    for ti, t0 in enumerate(range(0, L, TT)):
        f_ = ft[ti % 2]; tl_ = tl[ti % 2]; h_ = hid[ti % 2]
        p.dma("sp", f_[:, :], featsT_d[:, t0:t0 + TT], w=[f_])
        p.dma("sp", tl_[:, :], tlin_d[:, t0:t0 + TT], w=[tl_])
        for layer in range(3):
            ph = ps_h[layer % 2]
            if layer == 0:
                p.op("pe", lambda e: e.matmul(ph[:, :], lhsT=w_in[:, :], rhs=f_[:, :], start=True, stop=True), w=[ph], r=[w_in, f_])
            else:
                p.op("pe", lambda e: e.matmul(ph[:, :], lhsT=w_mid[:, layer - 1, :], rhs=h_[:, :], start=True, stop=True), w=[ph], r=[w_mid, h_])
            p.op("dve", lambda e: e.tensor_scalar(out=h_[:, :], in0=ph[:, :], scalar1=fs[:, 0:1], scalar2=fbs[:, layer:layer + 1],
                                                  op0=ALU.mult, op1=ALU.add), w=[h_], r=[ph, fs, fbs])
            p.op("dve", lambda e: e.tensor_copy(out=ki[:, :], in_=h_[:, :]), w=[ki], r=[h_])
            p.op("dve", lambda e: e.tensor_copy(out=kf[:, :], in_=ki[:, :]), w=[kf], r=[ki])
            p.op("dve", lambda e: e.tensor_tensor(out=h_[:, :], in0=h_[:, :], in1=kf[:, :], op=ALU.subtract), w=[h_], r=[h_, kf])
            p.op("dve", lambda e: e.tensor_scalar(out=kf[:, :], in0=h_[:, :], scalar1=0.0, scalar2=None, op0=ALU.is_lt), w=[kf], r=[h_])
            p.op("dve", lambda e: e.tensor_tensor(out=h_[:, :], in0=h_[:, :], in1=kf[:, :], op=ALU.add), w=[h_], r=[h_, kf])
            p.op("act", lambda e: e.activation(out=h_[:, :], in_=h_[:, :], func=AF.Sin, scale=2.0 * PI, bias=mpi[:, 0:1]), w=[h_], r=[h_, mpi])
        for rc in range(NR // 128):
            po = ps_o[rc % 2]; d_ = dec[rc % 2]; o_ = ot[oi % 3]; oi += 1
            p.op("pe", lambda e: e.matmul(po[:, :], lhsT=w_out[:, rc * 128:(rc + 1) * 128], rhs=h_[:, :], start=True, stop=True),
                 w=[po], r=[w_out, h_])
            p.op("act", lambda e: e.activation(out=d_[:, :], in_=tl_[:, :], func=AF.Exp, scale=dln[:, rc:rc + 1]), w=[d_], r=[tl_, dln])
            p.op("dve", lambda e: e.tensor_tensor(out=o_[:, :], in0=po[:, :], in1=d_[:, :], op=ALU.mult), w=[o_], r=[po, d_])
            p.dma("sp", out[rc * 128:(rc + 1) * 128, t0:t0 + TT], o_[:, :], r=[o_], is_output=True)
    return p.finish()


def hyfilt_inputs(w_in, b_in, w_mid, b_mid, w_out, freq, deltas):
    featsT, tlin = hy_feats()
    maps = []
    for i in range(8):
        cols = np.concatenate([n * 4096 + dr * 2048 + 256 * i + np.arange(256) for n in range(2) for dr in range(2)])
        m = {"featsT": featsT, "tlin": tlin, "w_in": np.ascontiguousarray(w_in), "w_mid": np.ascontiguousarray(w_mid),
             "w_out": np.ascontiguousarray(w_out[:, cols]),
             "vec": np.ascontiguousarray(np.stack([freq, b_in, b_mid[0], b_mid[1]], axis=1)),
             "deltas": np.ascontiguousarray(deltas[cols].reshape(8, 128).T)}
        maps.append(m)
    return maps


NB = L // 128
GW_ = 127 * 128
GPAD = 2 * L


def build_hyconv(T=L):
    p = Prog()
    pT_d = p.din("pT", [3, 256, T + 2]); cw_d = p.din("cw", [128, 3, 2, 3]); cb_d = p.din("cb", [128, 3, 2])
    G_d = p.din("G", [2, 256, GPAD]); skf_d = p.din("skf", [128, 2, 2, 2]); ident_d = p.din("ident", [128, 128])
    y = p.dout("y", [256, T])
    cw = p.sb([128, 3, 2, 3], F32, "cw"); cb = p.sb([128, 3, 2], F32, "cb"); skf = p.sb([128, 2, 2, 2], F32, "skf")
    ident = p.sb([128, 128], F32, "ident")
    p.dma("sp", cw[:, :, :, :], cw_d, w=[cw]); p.dma("sp", cb[:, :, :], cb_d, w=[cb]); p.dma("sp", skf[:, :, :, :], skf_d, w=[skf])
    p.dma("sp", ident[:, :], ident_d, w=[ident])
    sks = p.sb([128, 2, 2], F32, "sks")
    p.op("dve", lambda e: e.tensor_tensor(out=sks[:, :, :], in0=skf[:, 0, :, :], in1=skf[:, 1, :, :], op=ALU.add), w=[sks], r=[skf])
    Z = p.sb([128, T], F32, "Z"); ZT = p.sb([128, NB, 128], BF16, "ZT"); YT = p.sb([128, NB, 128], F32, "YT")
    gsk = [p.sb([128, GW_], BF16, f"gsk{i}") for i in range(2)]
    CT = 1024
    raw = [p.sb([128, CT + 2], F32, f"raw{i}") for i in range(2)]
    gt = [p.sb([128, CT], F32, f"gt{i}") for i in range(2)]
    vt = [p.sb([128, 128], F32, f"vt{i}") for i in range(2)]
    ps_t = [p.ps([128, 128], F32, f"ps_t{i}") for i in range(2)]
    ps_y = [p.ps([128, NB], F32, f"ps_y{i}") for i in range(4)]
    ri = [0]

    def conv_part(part, cg, t0, dst_ap, dst_buf):
        r_ = raw[ri[0] % 2]; ri[0] += 1
        p.dma("sp", r_[:, :], pT_d[part, cg * 128:(cg + 1) * 128, t0:t0 + CT + 2], w=[r_])
        p.op("dve", lambda e: e.tensor_scalar(out=dst_ap, in0=r_[:, 0:CT], scalar1=cw[:, part, cg, 0:1], scalar2=cb[:, part, cg:cg + 1],
                                              op0=ALU.mult, op1=ALU.add), w=[dst_buf], r=[r_, cw, cb])
        for j in (1, 2):
            p.op("dve", lambda e: e.scalar_tensor_tensor(out=dst_ap, in0=r_[:, j:j + CT], scalar=cw[:, part, cg, j:j + 1], in1=dst_ap,
                                                         op0=ALU.mult, op1=ALU.add), w=[dst_buf], r=[r_, cw, dst_buf])

    gi = 0
    for cg in range(2):
        for t0 in range(0, T, CT):
            conv_part(2, cg, t0, Z[:, t0:t0 + CT], Z)
        for n in range(2):
            for si in range(NB):
                pt_ = ps_t[si % 2]
                zin = Z[:, si * 128:(si + 1) * 128][:, ::-1]
                zr_ = vt[si % 2]
                p.op("pool", lambda e: e.tensor_copy(out=zr_[:, :], in_=zin), w=[zr_], r=[Z])
                p.op("pe", lambda e: e.transpose(pt_[:, :], zr_[:, :], ident[:, :]), w=[pt_], r=[zr_, ident])
                if si % 2:
                    p.op("act", lambda e: e.activation(out=ZT[:, si, :], in_=pt_[:, :], func=AF.Copy), w=[ZT], r=[pt_])
                else:
                    p.op("dve", lambda e: e.tensor_copy(out=ZT[:, si, :], in_=pt_[:, :]), w=[ZT], r=[pt_])
            for c in range(128):
                g_ = gsk[gi % 2]; py = ps_y[gi % 4]; gi += 1
                ch = cg * 128 + c
                src = bass.AP(tensor=G_d.tensor, offset=(n * 256 + ch) * GPAD, ap=[[1, 128], [1, GW_]])
                p.dma("pool", g_[:, :], src, w=[g_])
                ds = [0] + [s * k for k in range(1, NB) for s in (1, -1)]
                for idx, dd in enumerate(ds):
                    ti0 = max(0, dd); ti1 = min(NB, NB + dd)
                    si0 = ti0 - dd; si1 = ti1 - dd
                    p.op("pe", lambda e: e.matmul(py[:, ti0:ti1], lhsT=g_[:, 128 * (dd + 63):128 * (dd + 64)], rhs=ZT[:, si0:si1, c],
                                                  start=(idx == 0), stop=(idx == len(ds) - 1)), w=[py], r=[g_, ZT])
                if c % 2:
                    p.op("act", lambda e: e.activation(out=YT[:, :, c], in_=py[:, :], func=AF.Copy), w=[YT], r=[py])
                else:
                    p.op("dve", lambda e: e.tensor_copy(out=YT[:, :, c], in_=py[:, :]), w=[YT], r=[py])
            for t0 in range(0, T, CT):
                g2 = gt[(t0 // CT) % 2]
                conv_part(n, cg, t0, g2[:, :], g2)
                for tb in range(CT // 128):
                    ti = t0 // 128 + tb
                    pt_ = ps_t[ti % 2]; v_ = vt[ti % 2]
                    ts_ = slice(ti * 128, (ti + 1) * 128)
                    p.op("pe", lambda e: e.transpose(pt_[:, :], YT[:, ti, :], ident[:, :]), w=[pt_], r=[YT, ident])
                    p.op("dve", lambda e: e.scalar_tensor_tensor(out=v_[:, :], in0=Z[:, ts_], scalar=sks[:, n, cg:cg + 1], in1=pt_[:, :],
                                                                 op0=ALU.mult, op1=ALU.add), w=[v_], r=[Z, sks, pt_])
                    p.op("pool", lambda e: e.tensor_tensor(out=Z[:, ts_], in0=v_[:, :], in1=g2[:, tb * 128:(tb + 1) * 128], op=ALU.mult),
                         w=[Z], r=[v_, g2])
        for t0 in range(0, T, 2048):
            p.dma("sp", y[cg * 128:(cg + 1) * 128, t0:t0 + 2048], Z[:, t0:t0 + 2048], r=[Z], is_output=True)
    return p.finish()


def hyconv_inputs(P1, filt_rows, conv_w, conv_b, skip):
    ident = np.eye(128, dtype=np.float32)
    maps = []
    T = P1.shape[0]
    for i in range(8):
        m = {"ident": ident}
        pT = np.zeros((3, 256, T + 2), np.float32)
        cwl = np.zeros((128, 3, 2, 3), np.float32); cbl = np.zeros((128, 3, 2), np.float32)
        for part in range(3):
            cols = 4096 + part * 2048 + 256 * i + np.arange(256)
            pT[part, :, 1:T + 1] = P1[:, cols].T
            wc = conv_w[:, cols - 4096]
            cwl[:, part, :, :] = wc.reshape(3, 2, 128).transpose(2, 1, 0)
            cbl[:, part, :] = conv_b[cols - 4096].reshape(2, 128).T
        m["pT"] = pT; m["cw"] = cwl; m["cb"] = cbl
        f = filt_rows[i].reshape(2, 2, 256, L)
        G = np.zeros((2, 256, GPAD), np.float32)
        G[:, :, 0:L - 1] = f[:, 1, :, :0:-1]
        G[:, :, L - 1:2 * L - 1] = f[:, 0, :, :]
        m["G"] = G
        skf = np.zeros((128, 2, 2, 2), np.float32)
        skf[:, 0] = skip[:, 256 * i:256 * i + 256].reshape(2, 2, 128).transpose(2, 0, 1)
        skf[:, 1] = f[:, 1, :, 0].reshape(2, 2, 128).transpose(2, 0, 1)
        m["skf"] = skf
        maps.append(m)
    return maps


_PROGS = {}


def _prog(key, builder):
    if key not in _PROGS:
        _PROGS[key] = builder()
    return _PROGS[key]


def _run_inproj(xfull, cfull, w, nw, mo, moc):
    N = w.shape[1]
    nc = build_inproj(N)
    maps = []
    for i in range(8):
        xT = np.concatenate([xfull[i * 1024:(i + 1) * 1024], cfull[i * 32:(i + 1) * 32]], 0).T
        maps.append({"xT": np.ascontiguousarray(xT), "w": w, "nw": fm(nw), "scx": fm(mo[1]), "shx": fm(mo[0]),
                     "scc": fm(moc[1]), "shc": fm(moc[0])})
    res = run(nc, maps)
    Px = np.concatenate([r["yT"][:, :1024].T for r in res], 0)
    Pc = np.concatenate([r["yT"][:, 1024:].T for r in res], 0)
    return np.ascontiguousarray(Px), np.ascontiguousarray(Pc)


def kernel(**inp):
    g = {k: np.asarray(v) for k, v in inp.items()}
    x = g["x"][0].astype(np.float32)
    ctx = g["ctx"][0].astype(np.float32)
    mod = run_ada(g["c"], g["c_ctx"], g["ada_w"], g["ada_b"])
    mv = lambda l, r: [mod[l, r, j * D:(j + 1) * D] for j in range(6)]
    mo, moc = mv(0, 0), mv(0, 1)
    Px, Pc = _run_inproj(x, ctx, np.ascontiguousarray(g["ev_in_w"][0]), g["norm_mix_w"][0], mo, moc)
    res = run(build_ssd(256, 8192), ssd_inputs(Pc, Px, g["ev_ssd_conv_w"][0], g["ev_ssd_conv_b"][0], g["ev_ssd_dt_bias"][0],
                                               g["ev_ssd_a_log"][0], g["ev_ssd_d"][0]))
    ypx = np.concatenate([r["y_x"] for r in res], 1); ypc = np.concatenate([r["y_c"] for r in res], 1)
    res = run(build_na(), na_inputs(Pc, Px, g["ev_na_rpb"][0]))
    ynx = np.concatenate([r["y"] for r in res], 1); ync = np.concatenate([r["yc"] for r in res], 1)
    del Px, Pc
    tiles0 = [(0, 384, [(0, 384, "x")]), (384, 384, [(0, 384, "x")]), (768, 288, [(0, 256, "x"), (256, 288, "c")])]
    maps = []
    for i in range(8):
        sx = slice(i * 1024, (i + 1) * 1024); sc_ = slice(i * 32, (i + 1) * 32)
        mix = np.concatenate([np.concatenate([ypx[sx], ynx[sx]], 1), np.concatenate([ypc[sc_], ync[sc_]], 1)], 0)
        xx = np.concatenate([x[sx], ctx[sc_]], 0)
        m = {"mixT": np.ascontiguousarray(mix.T), "xT": np.ascontiguousarray(xx.T), "out_w": g["ev_out_w"][0],
             "w1": g["ev_ffn_w1"][0], "w3": g["ev_ffn_w3"][0], "w2": g["ev_ffn_w2"][0], "nwf": fm(g["norm_ffn_w"][0]),
             "ssd_nw": fm(np.concatenate([g["ev_ssd_norm_w"][0], g["ev_ssd_norm_w"][0]]))}
        for s, mm in (("x", mo), ("c", moc)):
            m[f"gmsa_{s}"] = fm(mm[2]); m[f"shm_{s}"] = fm(mm[3]); m[f"scm_{s}"] = fm(mm[4]); m[f"gmlp_{s}"] = fm(mm[5])
        maps.append(m)
    res = run(build_tail("dense", tiles0, 1056, gn=True, final=False), maps)
    x1 = np.ascontiguousarray(np.concatenate([r["xo"][:, :1024].T for r in res], 0))
    c1 = np.ascontiguousarray(np.concatenate([r["xo"][:, 1024:].T for r in res], 0))
    del maps, ypx, ypc, ynx, ync
    mo, moc = mv(1, 0), mv(1, 1)
    P1, P1c = _run_inproj(x1, c1, np.ascontiguousarray(g["od_in_w"][0]), g["norm_mix_w"][1], mo, moc)
    res = run(build_rg(), rg_inputs(np.ascontiguousarray(P1c[:, 2048:4096]), P1, g["od_rg_conv_w"][0], g["od_rg_conv_b"][0],
                                    g["od_rg_gate_w"][0], g["od_rg_gate_b"][0], g["od_rg_lambda"][0]))
    yrg = np.concatenate([r["y"].T for r in res], 1)
    res = run(build_hyfilt(), hyfilt_inputs(g["od_hy_w_in"][0], g["od_hy_b_in"][0], g["od_hy_w_mid"][0], g["od_hy_b_mid"][0],
                                            g["od_hy_w_out"][0], g["od_hy_freq"][0], g["od_hy_deltas"][0]))
    filt_rows = [r["filt"] for r in res]
    res = run(build_hyconv(), hyconv_inputs(P1, filt_rows, g["od_hy_conv_w"][0], g["od_hy_conv_b"][0], g["od_hy_skip"][0]))
    yhy = np.concatenate([r["y"].T for r in res], 1)
    del P1, P1c, filt_rows
    tiles1 = [(0, 384, [(0, 384, "x")]), (384, 384, [(0, 384, "x")]), (768, 256, [(0, 256, "x")])]
    sel = np.zeros((8, 8, 128), np.float32)
    for e in range(8):
        sel[e, e, :] = 1.0
    maps = []
    for i in range(8):
        sx = slice(i * 1024, (i + 1) * 1024)
        mix = np.concatenate([yrg[sx], yhy[sx]], 1)
        m = {"mixT": np.ascontiguousarray(mix.T), "xT": np.ascontiguousarray(x1[sx].T), "out_w": g["od_out_w"][0],
             "nwf": fm(g["norm_ffn_w"][1]),
             "rw": np.ascontiguousarray(g["od_router_w"][0].reshape(KC, 128, 8).transpose(1, 0, 2)),
             "sel": sel, "ident": np.eye(128, dtype=np.float32)}
        for s in ("x", "c"):
            m[f"gmsa_{s}"] = fm(mo[2]); m[f"shm_{s}"] = fm(mo[3]); m[f"scm_{s}"] = fm(mo[4]); m[f"gmlp_{s}"] = fm(mo[5])
        maps.append(m)
    res = run(build_tail("moe_pre", tiles1, 1024, gn=False, final=False), maps)
    x1T = [r["xo"] for r in res]
    hT = np.ascontiguousarray(np.concatenate([r["ho"] for r in res], 1))
    gates = np.concatenate([r["go"] for r in res], 1)
    maps = []
    for e in range(8):
        maps.append({"hT": hT, "g": np.ascontiguousarray(np.broadcast_to(gates[e][None], (128, 8192))),
                     "w1": g["od_moe_w1"][0][e], "w3": g["od_moe_w3"][0][e], "w2": g["od_moe_w2"][0][e]})
    res = run(build_expert(), maps)
    yparts = [r["yT"] for r in res]
    maps = []
    for i in range(8):
        sx = slice(i * 1024, (i + 1) * 1024)
        maps.append({"parts": np.ascontiguousarray(np.stack([yp[:, sx] for yp in yparts], 0)), "x1T": x1T[i],
                     "gmlp": fm(mo[5]), "fnw": fm(g["norm_out_w"])})
    res = run(build_combine(), maps)
    out = np.concatenate([r["xo"].T for r in res], 0)
    return np.ascontiguousarray(out[None]).astype(np.float32)
```
